# Optimizing a Trainium2 kernel written in Bass

```python
import math
import jax
import jax.numpy as jnp
from jax import lax
import numpy as np

D_MODEL = 2048
BATCH = 1
SEQ = 8192
DEPTH = 4

NH_M = 8
DH_M = D_MODEL // NH_M
DV_M = D_MODEL // NH_M
CONV_K = 4
CHUNK = 128
DH_A = 128
NH_A = D_MODEL // DH_A
NKV_A = NH_A // 4
NH_I = 16
DI = 64
TOPK_MAX = 256
QBLK = 128
N_BUCKETS = 32
MAX_DIST = 128
N_GROUPS = 4
EXP_PER_GROUP = 8
N_EXPERTS = N_GROUPS * EXP_PER_GROUP
TOP_K = 2
D_FF_E = D_MODEL // 4
MOE_BLK = 128
EPS = 1e-6

IN_SIZES = (2 * NH_M * DH_M, NH_M * DV_M, NH_M * DV_M, NH_M, NH_M,
            NH_A * DH_A, NKV_A * DH_A, NKV_A * DH_A,
            NH_I * DI, DI, NH_I, D_MODEL, D_MODEL)
D_IN = sum(IN_SIZES)

kernel_name = "hybrid_mlstm_dsa_hiermoe_trunk"


def rms(x):
    xf = x.astype(jnp.float32)
    return (xf * lax.rsqrt(jnp.mean(xf * xf, axis=-1, keepdims=True) + EPS)).astype(x.dtype)


def causal_conv(u, w):
    S = u.shape[1]
    K = w.shape[0]
    up = jnp.pad(u, ((0, 0), (K - 1, 0), (0, 0)))
    out = up[:, 0:S] * w[0]
    for j in range(1, K):
        out = out + up[:, j:j + S] * w[j]
    return out


def t5_bucket(dist):
    n = jnp.maximum(dist, 0)
    max_exact = N_BUCKETS // 2
    nf = jnp.maximum(n, max_exact).astype(jnp.float32)
    large = max_exact + (jnp.log(nf / max_exact) / math.log(MAX_DIST / max_exact)
                         * (N_BUCKETS - max_exact)).astype(jnp.int32)
    large = jnp.minimum(large, N_BUCKETS - 1)
    return jnp.where(n < max_exact, n, large)


def mlstm(q, k, v, i_pre, f_pre):
    B, S, H, dk = q.shape
    dv = v.shape[-1]
    L = CHUNK
    NC = S // L
    f32 = jnp.float32

    def chunks(t):
        return t.astype(f32).reshape(B, NC, L, H, -1).transpose(0, 3, 1, 2, 4)

    qc = chunks(q)
    kc = chunks(k) * (dk ** -0.5)
    vc = chunks(v)
    ig = i_pre.astype(f32).reshape(B, NC, L, H).transpose(0, 3, 1, 2)
    lf = jax.nn.log_sigmoid(f_pre.astype(f32)).reshape(B, NC, L, H).transpose(0, 3, 1, 2)
    b = jnp.cumsum(lf, axis=-1)
    g = b[..., -1]
    causal = jnp.tril(jnp.ones((L, L), bool))
    dmat = jnp.where(causal, b[..., :, None] - b[..., None, :] + ig[..., None, :], -jnp.inf)
    m_intra = jnp.max(dmat, axis=-1)
    a = g[..., None] - b + ig
    m_loc = jnp.max(a, axis=-1)
    w_loc = jnp.exp(a - m_loc[..., None])
    c_loc = jnp.einsum('bhcld,bhcle->bhcde', kc * w_loc[..., None], vc)
    n_loc = jnp.einsum('bhcl,bhcld->bhcd', w_loc, kc)

    def step(carry, inp):
        c_st, n_st, m_st = carry
        c_l, n_l, m_l, g_c = inp
        m_new = jnp.maximum(g_c + m_st, m_l)
        s_p = jnp.exp(g_c + m_st - m_new)
        s_l = jnp.exp(m_l - m_new)
        c_new = s_p[..., None, None] * c_st + s_l[..., None, None] * c_l
        n_new = s_p[..., None] * n_st + s_l[..., None] * n_l
        return (c_new, n_new, m_new), (c_st, n_st, m_st)

    init = (jnp.zeros((B, H, dk, dv), f32), jnp.zeros((B, H, dk), f32), jnp.zeros((B, H), f32))
    xs = (c_loc.transpose(2, 0, 1, 3, 4), n_loc.transpose(2, 0, 1, 3),
          m_loc.transpose(2, 0, 1), g.transpose(2, 0, 1))
    _, (c_prev, n_prev, m_prev) = lax.scan(step, init, xs)
    c_prev = c_prev.transpose(1, 2, 0, 3, 4)
    n_prev = n_prev.transpose(1, 2, 0, 3)
    m_prev = m_prev.transpose(1, 2, 0)
    inter = b + m_prev[..., None]
    m_t = jnp.maximum(inter, m_intra)
    s_inter = jnp.exp(inter - m_t)
    w_intra = jnp.exp(dmat - m_t[..., None]) * jnp.einsum('bhcld,bhcsd->bhcls', qc, kc)
    num = (jnp.einsum('bhcls,bhcse->bhcle', w_intra, vc)
           + s_inter[..., None] * jnp.einsum('bhcld,bhcde->bhcle', qc, c_prev))
    den = jnp.sum(w_intra, axis=-1) + s_inter * jnp.einsum('bhcld,bhcd->bhcl', qc, n_prev)
    h = num / jnp.maximum(jnp.abs(den), jnp.exp(-m_t))[..., None]
    return h.transpose(0, 2, 3, 1, 4).reshape(B, S, H, dv).astype(v.dtype)


def dsa_attention(q, k, v, q_idx, k_idx, w_idx, rel_bias):
    B, S, H, dh = q.shape
    hkv = k.shape[2]
    grp = H // hkv
    topk = min(TOPK_MAX, S // 4)
    nb = S // QBLK
    key_pos = jnp.arange(S, dtype=jnp.int32)
    bidx = jnp.arange(B)[:, None, None]

    def block(args):
        qb, qib, wb, t0 = args
        tq = t0 + jnp.arange(QBLK, dtype=jnp.int32)
        sc = jax.nn.relu(jnp.einsum('bqhd,bsd->bqhs', qib, k_idx))
        sc = jnp.einsum('bqhs,bqh->bqs', sc, wb).astype(jnp.float32)
        sc = jnp.where((key_pos[None, :] <= tq[:, None])[None], sc, -jnp.inf)
        _, idx = lax.top_k(sc, topk)
        ks = k[bidx, idx]
        vs = v[bidx, idx]
        logits = jnp.einsum('bqngd,bqknd->bqngk', qb.reshape(B, QBLK, hkv, grp, dh),
                            ks).astype(jnp.float32) * (dh ** -0.5)
        dist = tq[None, :, None] - idx
        bias = rel_bias[t5_bucket(dist)].astype(jnp.float32)
        bias = bias.reshape(B, QBLK, topk, hkv, grp).transpose(0, 1, 3, 4, 2)
        valid = (dist >= 0)[:, :, None, None, :]
        p = jax.nn.softmax(jnp.where(valid, logits + bias, -jnp.inf), axis=-1)
        out = jnp.einsum('bqngk,bqknd->bqngd', p.astype(vs.dtype), vs)
        return out.reshape(B, QBLK, H, dh)

    def blocks(t):
        return t.reshape(B, nb, QBLK, *t.shape[2:]).swapaxes(0, 1)

    out = lax.map(block, (blocks(q), blocks(q_idx), blocks(w_idx),
                          jnp.arange(nb, dtype=jnp.int32) * QBLK))
    return out.swapaxes(0, 1).reshape(B, S, H * dh)


def hybrid_mixer(h, w_in, conv_w, b_gate, mlstm_norm, q_norm, k_norm, rel_bias,
                 w_proj_m, w_proj_a, w_out):
    B, S, _ = h.shape
    z = h @ w_in
    cuts = [int(t) for t in np.cumsum(IN_SIZES)[:-1]]
    qk_raw, vm, om, ip, fp, qa, ka, va, qi, ki, wi, gm, ga = jnp.split(z, cuts, axis=-1)
    qk = jax.nn.silu(causal_conv(qk_raw, conv_w))
    qm, km = jnp.split(qk, 2, axis=-1)
    ip = ip + b_gate[:NH_M]
    fp = fp + b_gate[NH_M:]
    hm = mlstm(qm.reshape(B, S, NH_M, DH_M), km.reshape(B, S, NH_M, DH_M),
               vm.reshape(B, S, NH_M, DV_M), ip, fp)
    ym = rms(hm).reshape(B, S, NH_M * DV_M) * mlstm_norm * jax.nn.sigmoid(om)
    qa = rms(qa.reshape(B, S, NH_A, DH_A)) * q_norm
    ka = rms(ka.reshape(B, S, NKV_A, DH_A)) * k_norm
    va = va.reshape(B, S, NKV_A, DH_A)
    wi = wi * ((NH_I * DI) ** -0.5)
    ya = dsa_attention(qa, ka, va, qi.reshape(B, S, NH_I, DI), ki, wi, rel_bias)
    merged = jax.nn.sigmoid(gm) * (ym @ w_proj_m) + jax.nn.sigmoid(ga) * (ya @ w_proj_a)
    return merged @ w_out


def hier_moe(h, w_grp, b_grp, w_exp, b_exp, w1, w3, w2):
    B, S, D = h.shape
    n = B * S
    hf = h.reshape(n, D)
    g_logit = (hf @ w_grp + b_grp).astype(jnp.float32)
    g_sel = jnp.argmax(g_logit, axis=-1)
    p_grp = jnp.take_along_axis(jax.nn.softmax(g_logit, axis=-1), g_sel[:, None], axis=-1)
    e_logit = (hf @ w_exp + b_exp).astype(jnp.float32).reshape(n, N_GROUPS, EXP_PER_GROUP)
    e_logit = jnp.take_along_axis(e_logit, g_sel[:, None, None], axis=1)[:, 0]
    top_v, top_i = lax.top_k(e_logit, TOP_K)
    gate = p_grp * jax.nn.softmax(top_v, axis=-1)
    expert = (g_sel[:, None] * EXP_PER_GROUP + top_i).reshape(-1).astype(jnp.int32)
    m = n * TOP_K
    tok = jnp.repeat(jnp.arange(n, dtype=jnp.int32), TOP_K)
    order = jnp.argsort(expert)
    e_s = expert[order]
    tok_s = tok[order]
    gate_s = gate.reshape(-1)[order]
    counts = jnp.bincount(expert, length=N_EXPERTS)
    offs = jnp.cumsum(counts) - counts
    pcounts = (counts + MOE_BLK - 1) // MOE_BLK * MOE_BLK
    pends = jnp.cumsum(pcounts)
    dest = pends[e_s] - pcounts[e_s] + jnp.arange(m, dtype=jnp.int32) - offs[e_s]
    nblk = -(-m // MOE_BLK) + N_EXPERTS
    rows = nblk * MOE_BLK
    row_tok = jnp.zeros((rows,), jnp.int32).at[dest].set(tok_s)
    row_gate = jnp.zeros((rows,), h.dtype).at[dest].set(gate_s.astype(h.dtype))
    blk_exp = jnp.minimum(jnp.searchsorted(pends, jnp.arange(nblk, dtype=jnp.int32) * MOE_BLK,
                                           side='right'), N_EXPERTS - 1)

    def run(args):
        e, toks = args
        xb = hf[toks]
        return (jax.nn.silu(xb @ w1[e]) * (xb @ w3[e])) @ w2[e]

    yb = lax.map(run, (blk_exp, row_tok.reshape(nblk, MOE_BLK)))
    y = jax.ops.segment_sum(yb.reshape(rows, D) * row_gate[:, None], row_tok, num_segments=n)
    return y.reshape(B, S, D)


def setup_inputs(seed: int = 0) -> dict:
    key = jax.random.key(seed)
    ks = jax.random.split(key, 24)
    D = D_MODEL

    def nrm(k, shape, s):
        return jax.random.normal(k, shape, jnp.float32) * s

    b_gate = jnp.concatenate([nrm(ks[8], (DEPTH, NH_M), 0.1),
                              jnp.linspace(3.0, 6.0, NH_M, dtype=jnp.float32)[None]
                              + nrm(ks[9], (DEPTH, NH_M), 0.1)], axis=-1)
    return {
        "x": nrm(ks[0], (BATCH, SEQ, D), 1.0),
        "c": nrm(ks[1], (BATCH, D), 1.0),
        "w_ada": nrm(ks[2], (DEPTH, D, 6 * D), 0.5 * D ** -0.5),
        "b_ada": nrm(ks[3], (DEPTH, 6 * D), 0.01),
        "norm1": 1.0 + nrm(ks[4], (DEPTH, D), 0.02),
        "norm2": 1.0 + nrm(ks[5], (DEPTH, D), 0.02),
        "w_in": nrm(ks[6], (DEPTH, D, D_IN), D ** -0.5),
        "conv_w": nrm(ks[7], (DEPTH, CONV_K, 2 * NH_M * DH_M), CONV_K ** -0.5),
        "b_gate": b_gate,
        "mlstm_norm": 1.0 + nrm(ks[10], (DEPTH, NH_M * DV_M), 0.02),
        "q_norm": 1.0 + nrm(ks[11], (DEPTH, DH_A), 0.02),
        "k_norm": 1.0 + nrm(ks[12], (DEPTH, DH_A), 0.02),
        "rel_bias": nrm(ks[13], (N_BUCKETS, NH_A), 0.5),
        "w_proj_m": nrm(ks[14], (DEPTH, NH_M * DV_M, D), (NH_M * DV_M) ** -0.5),
        "w_proj_a": nrm(ks[15], (DEPTH, NH_A * DH_A, D), (NH_A * DH_A) ** -0.5),
        "w_out": nrm(ks[16], (DEPTH, D, D), D ** -0.5),
        "w_grp": nrm(ks[17], (DEPTH, D, N_GROUPS), D ** -0.5),
        "b_grp": nrm(ks[18], (DEPTH, N_GROUPS), 0.01),
        "w_exp": nrm(ks[19], (DEPTH, D, N_EXPERTS), D ** -0.5),
        "b_exp": nrm(ks[20], (DEPTH, N_EXPERTS), 0.01),
        "w1": nrm(ks[21], (DEPTH, N_EXPERTS, D, D_FF_E), D ** -0.5),
        "w3": nrm(ks[22], (DEPTH, N_EXPERTS, D, D_FF_E), D ** -0.5),
        "w2": nrm(ks[23], (DEPTH, N_EXPERTS, D_FF_E, D), D_FF_E ** -0.5),
    }


def reference(x, c, w_ada, b_ada, norm1, norm2, w_in, conv_w, b_gate, mlstm_norm, q_norm,
              k_norm, rel_bias, w_proj_m, w_proj_a, w_out, w_grp, b_grp, w_exp, b_exp,
              w1, w3, w2):
    c_act = jax.nn.silu(c)
    for l in range(DEPTH):
        mod = c_act @ w_ada[l] + b_ada[l]
        sh1, sc1, g1, sh2, sc2, g2 = jnp.split(mod[:, None, :], 6, axis=-1)
        h = rms(x) * norm1[l] * (1.0 + sc1) + sh1
        x = x + g1 * hybrid_mixer(h, w_in[l], conv_w[l], b_gate[l], mlstm_norm[l], q_norm[l],
                                  k_norm[l], rel_bias, w_proj_m[l], w_proj_a[l], w_out[l])
        h = rms(x) * norm2[l] * (1.0 + sc2) + sh2
        x = x + g2 * hier_moe(h, w_grp[l], b_grp[l], w_exp[l], b_exp[l], w1[l], w3[l], w2[l])
    return x
```

```python
import contextlib
import numpy as np
import concourse.bass as bass
import concourse.mybir as mybir
from concourse.bass_utils import run_bass_kernel_spmd

F32 = mybir.dt.float32
BF16 = mybir.dt.bfloat16
U32 = mybir.dt.uint32
AF = mybir.ActivationFunctionType
ALU = mybir.AluOpType
AX = mybir.AxisListType

NCORE = 8
D = 2048
KC = D // 128
NH_M, DH_M = 8, 256
DH_A, NH_A, NKV = 128, 16, 4
NH_I, DI = 16, 64
N_EXP, D_FF = 32, 512
EPS = 1e-6
IN_SIZES = (4096, 2048, 2048, 8, 8, 2048, 512, 512, 1024, 64, 16, 2048, 2048)
D_IN = sum(IN_SIZES)
OFF = {}
_o = 0
for _n, _s in zip(("qk", "vm", "om", "ip", "fp", "qa", "ka", "va", "qi", "ki", "wi", "gm", "ga"), IN_SIZES):
    OFF[_n] = _o
    _o += _s


class Buf:
    NID = 0

    def __init__(self, k, t, name, dram=False):
        self.k, self.t, self.name, self.dram = k, t, name, dram
        Buf.NID += 1
        self.uid = Buf.NID
        self.writer = None
        self.readers = []
        self.dsem = None
        self.dcnt = 0

    def __getitem__(self, idx):
        return self.t[idx]

    def ap(self):
        return self.t.ap() if self.dram else self.t[:]


class KB:
    def __init__(self, nc, st):
        self.nc, self.st = nc, st
        self.engs = {"pe": nc.tensor, "act": nc.scalar, "dve": nc.vector, "pool": nc.gpsimd, "sp": nc.sync}
        self.sem = {e: st.enter_context(nc.semaphore("sem_" + e)) for e in self.engs}
        self.cnt = {e: 0 for e in self.engs}
        self.waited = {e: {} for e in self.engs}
        self.prog = {e: [] for e in self.engs}
        self.meta = {e: [] for e in self.engs}
        self.cc_sem = st.enter_context(nc.semaphore("sem_cc"))
        self.cc_cnt = 0
        self.nbuf = 0
        self.scopes = [st]
        self.scope_bufs = [[]]
        self.dram_level = 0
        self.sem_free = []
        self.dma_toks = {}
        self.pid_cache = {}

    def pid(self, E):
        key = id(E)
        if key not in self.pid_cache:
            self.pid_cache[key] = E.partition_id()
        return self.pid_cache[key]

    def prev(self, E):
        key = ("prev", id(E))
        if key not in self.pid_cache:
            self.pid_cache[key] = E.snap((self.pid(E) + NCORE - 1) % NCORE, min_val=0, max_val=NCORE - 1)
        return self.pid_cache[key]

    def push(self, layer=False):
        s = contextlib.ExitStack()
        self.scopes.append(s)
        self.scope_bufs.append([])
        if layer:
            self.dram_level = len(self.scopes) - 1

    def pop(self, layer=False):
        self.barrier()
        for b in self.scope_bufs.pop():
            if b.dsem is not None:
                self.sem_free.append((b.dsem, b.dcnt))
                b.dsem = None
        self.scopes.pop().close()
        if layer:
            self.dram_level = 0

    def barrier(self):
        toks = [(e, self.sem[e], self.cnt[e]) for e in self.engs if self.cnt[e] > 0]
        toks += list(self.dma_toks.values())
        if self.cc_cnt:
            toks.append(("cc", self.cc_sem, self.cc_cnt))
        for e in self.engs:
            for tok in toks:
                if tok[0] != e:
                    self._wait(e, tok)

    def sb(self, name, shape, dt):
        self.nbuf += 1
        t = self.scopes[-1].enter_context(self.nc.sbuf_tensor(f"{name}_{self.nbuf}", list(shape), dt))
        b = Buf(self, t, name)
        self.scope_bufs[-1].append(b)
        return b

    def sb_main(self, name, shape, dt):
        self.nbuf += 1
        t = self.scopes[0].enter_context(self.nc.sbuf_tensor(f"{name}_{self.nbuf}", list(shape), dt))
        b = Buf(self, t, name)
        self.scope_bufs[0].append(b)
        return b

    def ps(self, name, shape, dt=F32):
        self.nbuf += 1
        t = self.st.enter_context(self.nc.psum_tensor(f"{name}_{self.nbuf}", list(shape), dt))
        return Buf(self, t, name)

    def dram(self, name, shape, dt, kind=None):
        if kind is None:
            t = self.nc.dram_tensor(name, list(shape), dt)
        else:
            t = self.nc.dram_tensor(name, list(shape), dt, kind=kind)
        b = Buf(self, t, name, dram=True)
        self.scope_bufs[self.dram_level].append(b)
        return b

    def _wait(self, e, tok):
        key, sem, val = tok
        if self.waited[e].get(key, 0) >= val:
            return
        self.waited[e][key] = val
        self.meta[e].append(("wait", id(sem), val))
        self.prog[e].append(lambda E, sem=sem, val=val: E.wait_ge(sem, val))

    def _deps(self, e, reads, writes, is_dma):
        toks = []
        for b in reads:
            if b.writer is not None:
                toks.append(b.writer)
        for b in writes:
            if b.writer is not None:
                toks.append(b.writer)
            toks.extend(b.readers)
        for tok in toks:
            if (not is_dma) and tok[0] == e and e == "pe":
                continue
            self._wait(e, tok)

    def op(self, e, fn, reads=(), writes=(), signal=True):
        self._deps(e, reads, writes, False)
        if signal:
            self.cnt[e] += 1
            n = self.cnt[e]
            sem = self.sem[e]
            self.prog[e].append(lambda E, fn=fn, sem=sem: fn(E).then_inc(sem, 1))
            self.meta[e].append(("inc", id(sem), 1))
            tok = (e, sem, n)
            for b in reads:
                b.readers.append(tok)
                if len(b.readers) > 6:
                    b.readers = b.readers[-6:] if all(r[0] == b.readers[-1][0] for r in b.readers) else b.readers
            for b in writes:
                b.writer = tok
                b.readers = []
        else:
            self.prog[e].append(lambda E, fn=fn: fn(E))

    def dma(self, q, out_ap, in_ap, reads, write, **kw):
        self._deps(q, reads, [write], True)
        if write.dsem is None:
            if self.sem_free:
                write.dsem, write.dcnt = self.sem_free.pop()
            else:
                write.dsem = self.st.enter_context(self.nc.semaphore("ds_%s_%d" % (write.name, write.uid)))
        write.dcnt += 16
        sem, val = write.dsem, write.dcnt
        self.meta[q].append(("inc", id(sem), 16))
        def emit(E, o=out_ap, i=in_ap, sem=sem, kw=kw, nm=write.name):
            try:
                return E.dma_start(out=(o(E) if callable(o) else o), in_=(i(E) if callable(i) else i), **kw).then_inc(sem, 16)
            except Exception as ex:
                raise RuntimeError("dma to %s failed: %r" % (nm, ex))
        self.prog[q].append(emit)
        tok = ("d%d" % write.uid, sem, val)
        self.dma_toks[tok[0]] = tok
        for b in reads:
            b.readers.append(tok)
        write.writer = tok
        write.readers = []

    def allgather(self, src, dst):
        self._deps("pool", [src], [dst], True)
        self.cc_cnt += 1
        sem, val = self.cc_sem, self.cc_cnt
        s_ap, d_ap = src.t.ap().opt(), dst.t.ap().opt()
        self.meta["pool"].append(("inc", id(sem), 1))
        self.prog["pool"].append(lambda E, s=s_ap, d=d_ap, sem=sem: E.collective_compute(
            "AllGather", ALU.bypass, replica_groups=[list(range(NCORE))], ins=[s], outs=[d]).then_inc(sem))
        tok = ("cc", sem, val)
        src.readers.append(tok)
        dst.writer = tok
        dst.readers = []

    def simulate(self):
        pos = {e: 0 for e in self.engs}
        sems = {}
        progress = True
        while progress:
            progress = False
            for e in self.engs:
                while pos[e] < len(self.meta[e]):
                    kind, sid, v = self.meta[e][pos[e]]
                    if kind == "wait":
                        if sems.get(sid, 0) < v:
                            break
                    else:
                        sems[sid] = sems.get(sid, 0) + v
                    pos[e] += 1
                    progress = True
        stuck = {e: (pos[e], len(self.meta[e]), self.meta[e][pos[e]]) for e in self.engs if pos[e] < len(self.meta[e])}
        return stuck

    def finish(self, block, final_bufs):
        for b in final_bufs:
            self._wait("sp", b.writer)
        kb = self

        @block.tensor
        def _(E):
            for f in kb.prog["pe"]:
                f(E)

        @block.scalar
        def _(E):
            for f in kb.prog["act"]:
                f(E)

        @block.vector
        def _(E):
            for f in kb.prog["dve"]:
                f(E)

        @block.gpsimd
        def _(E):
            for f in kb.prog["pool"]:
                f(E)

        @block.sync
        def _(E):
            for f in kb.prog["sp"]:
                f(E)


class Prog:
    def __init__(self, depth=4, S=8192, stop=None):
        self.depth, self.S, self.L, self.stop = depth, S, S // NCORE, stop
        self.taps = {}

    def rot(self, name, n, shape, dt):
        bufs = [self.k.sb(f"{name}{i}", shape, dt) for i in range(n)]
        state = {"i": 0}

        def nxt():
            b = bufs[state["i"] % n]
            state["i"] += 1
            return b
        return nxt

    def next_ps(self):
        b = self.psr[self.psi % len(self.psr)]
        self.psi += 1
        return b

    def next_q(self):
        self.qi += 1
        return ("sp", "act")[self.qi % 2]

    def cast_gather(self, name, src_dram, nelem_shard, out_shape, src_ap=None):
        k = self.k
        assert nelem_shard % 128 == 0
        F = nelem_shard // 128
        stage = k.dram(name + "_st", [128, F], BF16)
        full = k.dram(name + "_bf", [NCORE * 128, F], BF16)
        src2 = (src_ap if src_ap is not None else src_dram.t.ap()).rearrange("(p f) -> p f", p=128)
        CH = 4096
        for c0 in range(0, F, CH):
            w = min(CH, F - c0)
            a = self.cg_f32()
            b = self.cg_bf()
            k.dma(self.next_q(), a[:, 0:w], src2[:, c0:c0 + w], [src_dram], a)
            e = ("dve", "act")[self.cgi % 2]
            self.cgi += 1
            if e == "act":
                k.op(e, lambda E, o=b[:, 0:w], i=a[:, 0:w]: E.copy(out=o, in_=i), [a], [b])
            else:
                k.op(e, lambda E, o=b[:, 0:w], i=a[:, 0:w]: E.tensor_copy(out=o, in_=i), [a], [b])
            k.dma(self.next_q(), stage[:, c0:c0 + w], b[:, 0:w], [b], stage)
        k.allgather(stage, full)
        full.view = full.t.ap().rearrange("(r p) f -> (r p f)", p=128)
        full.shape2 = out_shape
        return full

    def cast_private(self, name, src_ap, src_buf, nelem):
        k = self.k
        F = nelem // 128
        dst = k.dram(name + "_pbf", [128, F], BF16)
        src2 = src_ap.rearrange("(p f) -> p f", p=128)
        CH = 4096
        for c0 in range(0, F, CH):
            w = min(CH, F - c0)
            a = self.cg_f32()
            b = self.cg_bf()
            k.dma(self.next_q(), a[:, 0:w], src2[:, c0:c0 + w], [src_buf], a)
            e = ("dve", "act")[self.cgi % 2]
            self.cgi += 1
            if e == "act":
                k.op(e, lambda E, o=b[:, 0:w], i=a[:, 0:w]: E.copy(out=o, in_=i), [a], [b])
            else:
                k.op(e, lambda E, o=b[:, 0:w], i=a[:, 0:w]: E.tensor_copy(out=o, in_=i), [a], [b])
            k.dma(self.next_q(), dst.t.ap()[:, c0:c0 + w], b[:, 0:w], [b], dst)
        return dst

    def wview(self, full, pattern, **kw):
        rows, cols = full.shape2
        v = full.t.ap().rearrange("(r p) f -> (r p f)", p=128).rearrange("(a b) -> a b", b=cols)
        return v

    def build(self):
        nc = bass.Bass("TRN2", target_bir_lowering=False)
        self.nc = nc
        depth, L = self.depth, self.L
        st = contextlib.ExitStack()
        self.st = st
        k = KB(nc, st)
        self.k = k
        self.psi = self.qi = self.cgi = 0
        ein = lambda name, shape, dt=F32: k.dram(name, shape, dt, kind="ExternalInput")
        self.xT_in = ein("xT", [D, L])
        self.cvec = ein("cvec", [128, KC])
        self.w_ada = ein("w_ada", [depth, D, 1536])
        self.b_ada = ein("b_ada", [128, depth * 12])
        self.norm1 = ein("norm1", [128, depth * KC])
        self.norm2 = ein("norm2", [128, depth * KC])
        self.w_in_sh = ein("w_in", [depth, (D // NCORE) * D_IN])
        self.wr_in = ein("wr", [depth, 128, KC * 36])
        self.br_in = ein("br", [depth, 128, 36])
        self.ident_in = ein("ident", [128, 128])
        self.esel_in = ein("esel", [32, 32 * 128])
        self.causal_in = ein("causalT", [128, 128])
        self.relb_in = ein("rel_bias", [32, 16])
        self.causaln_in = ein("causalN", [128, 256])
        self.hasprev_in = ein("hasprev", [128, 1])
        self.bkt_in = ein("bkt", [2, 32, 128 * 128])
        self.qpos_in = ein("qpos", [128, L // 128])
        self.kpos_in = ein("kpos", [128, 512])
        self.qn_in = ein("qn", [128, depth])
        self.kn_in = ein("kn", [128, depth])
        self.mn_in = ein("mn", [128, depth * KC])
        ws = (D // NCORE) * D
        self.wpm_sh = ein("w_proj_m", [depth, ws])
        self.wpa_sh = ein("w_proj_a", [depth, ws])
        self.wo_sh = ein("w_out", [depth, ws])
        self.whead_in = ein("whead", [depth, D * 1026])
        self.convh_in = ein("conv_h", [depth, 128, 16])
        self.bgh_in = ein("bg_h", [depth, 64, 2])
        ES = (N_EXP // NCORE) * D * D_FF
        self.w1_sh = ein("w1", [depth, ES])
        self.w3_sh = ein("w3", [depth, ES])
        self.w2_sh = ein("w2", [depth, ES])
        self.out = k.dram("out", [D, L], F32, kind="ExternalOutput")
        self.ones_col = k.sb("ones_col", [128, 1], F32)
        self.ones_row = k.sb("ones_row", [1, 128], F32)
        k.op("dve", lambda E: E.memset(self.ones_col[:], 1.0), [], [self.ones_col])
        k.op("dve", lambda E: E.memset(self.ones_row[:], 1.0), [], [self.ones_row])
        self.psr = [k.ps(f"ps{i}", [128, 512]) for i in range(3)]
        self.psb16 = k.ps("psb16", [128, 512], BF16)
        self.ident = k.sb("identf", [128, 128], F32)
        self.identb = k.sb("identb", [128, 128], BF16)
        self.causalT = k.sb("causalT", [128, 128], F32)
        k.dma("sp", self.ident[:], self.ident_in.t.ap(), [self.ident_in], self.ident)
        k.dma("act", self.causalT[:], self.causal_in.t.ap(), [self.causal_in], self.causalT)
        k.op("dve", lambda E: E.tensor_copy(out=self.identb[:], in_=self.ident[:]), [self.ident], [self.identb])
        self.eps128 = k.sb("eps128", [128, 1], F32)
        k.op("dve", lambda E: E.memset(self.eps128[:], EPS), [], [self.eps128])
        self.ps_aux = [k.ps(f"psx{i}", [128, 512]) for i in range(4)]
        self.xT = k.dram("xT_work", [D, L], F32)

        self.modsb = k.sb("modsb", [128, depth, NCORE, 12], F32)
        self.n1 = k.sb("n1", [128, depth * KC], F32)
        self.n2 = k.sb("n2", [128, depth * KC], F32)
        k.push()
        self.stage_mod()
        k.pop()
        if self.stop == "mod":
            return self.finish_debug()
        self.build_bias_tables()
        for l in range(depth):
            if self.stop is None:
                k.push(layer=True)
            self.layer(l)
            if self.stop is not None and l == 0:
                return self.finish_debug()
            k.pop(layer=True)
        k.dma("sp", self.out.t.ap(), self.xT.t.ap(), [self.xT], self.out)
        return self.finish_debug()

    def tap(self, name, buf_dram):
        self.taps[name] = buf_dram

    def finish_debug(self):
        k = self.k
        finals = []
        for name, b in self.taps.items():
            shp = list(b.t.shape)
            o = k.dram("tap_" + name, shp, b.t.dtype, kind="ExternalOutput")
            k.dma("sp", o.t.ap(), b.t.ap(), [b], o)
            finals.append(o)
        if self.out.writer is not None:
            finals.append(self.out)
        block = self.st.enter_context(self.nc.Block())
        k.finish(block, finals)
        self.st.close()
        return self.nc

    def stage_mod(self):
        k, depth = self.k, self.depth
        cv = k.sb("cv", [128, KC], F32)
        cact = k.sb("cact", [128, KC], F32)
        k.dma("sp", cv[:], self.cvec.t.ap(), [self.cvec], cv)
        k.op("act", lambda E: E.activation(out=cact[:], in_=cv[:], func=AF.Silu), [cv], [cact])
        wa = self.rot("wa", 2, [128, KC, 512], F32)
        psm = self.ps_aux[0]
        for l in range(depth):
            for g in range(3):
                w = wa()
                src = self.w_ada.t.ap()[l].rearrange("(kc p) n -> p kc n", p=128)[:, :, g * 512:(g + 1) * 512]
                k.dma(self.next_q(), w[:], src, [self.w_ada], w)
                for j4 in range(4):
                    col = l * 12 + g * 4 + j4
                    for kc in range(KC):
                        k.op("pe", lambda E, w=w, kc=kc, j4=j4, col=col: E.matmul(
                            psm[:, col:col + 1], w[:, kc, j4 * 128:(j4 + 1) * 128], cact[:, kc:kc + 1],
                            start=(kc == 0), stop=(kc == KC - 1)), [w, cact], [psm], signal=(kc == KC - 1))
        bsb = k.sb("bada", [128, depth * 12], F32)
        msb = k.sb("modsrc", [128, depth * 12], F32)
        k.dma("sp", bsb[:], self.b_ada.t.ap(), [self.b_ada], bsb)
        k.op("dve", lambda E: E.tensor_tensor(out=msb[:], in0=psm[:, 0:depth * 12], in1=bsb[:], op=ALU.add), [psm, bsb], [msb])
        msrc = k.dram("mod_src", [128, depth * 12], F32)
        mfull = k.dram("mod_full", [NCORE * 128, depth * 12], F32)
        k.dma("sp", msrc.t.ap(), msb[:], [msb], msrc)
        k.allgather(msrc, mfull)
        k.dma("sp", self.modsb[:], mfull.t.ap().rearrange("(r p) (l j) -> p l r j", p=128, j=12), [mfull], self.modsb,
              allow_slow_non_contiguous=True)
        self.tap("mod", mfull)
        k.dma("sp", self.n1[:], self.norm1.t.ap(), [self.norm1], self.n1)
        k.dma("act", self.n2[:], self.norm2.t.ap(), [self.norm2], self.n2)

    def modv(self, l, seg):
        return self.modsb[:, l].rearrange("p r j -> p (r j)")[:, seg * KC:(seg + 1) * KC]

    def norm_mod(self, l, which, src_dram, hT, hf_cb=None):
        k, L = self.k, self.L
        k.push()
        self.xt_rot = self.rot("xt", 2, [128, KC, 512], F32)
        self.sq_rot = self.rot("sq", 3, [128, 512], F32)
        self.rs_rot = self.rot("rs", 2, [1, 512], F32)
        gains = self.n1 if which == 0 else self.n2
        a = self.a_t[which]
        sc = self.modv(l, 1 + 3 * which)
        sh = self.modv(l, 0 + 3 * which)
        k.op("dve", lambda E: E.scalar_tensor_tensor(out=a[:], in0=sc, scalar=1.0, in1=gains[:, l * KC:(l + 1) * KC],
                                                     op0=ALU.add, op1=ALU.mult), [self.modsb, gains], [a])
        xsrc = src_dram.t.ap().rearrange("(fc p) t -> p fc t", p=128)
        for tt in range(L // 512):
            xt = self.xt_rot()
            k.dma(self.next_q(), xt[:], xsrc[:, :, tt * 512:(tt + 1) * 512], [src_dram], xt)
            pss = self.ps_aux[1]
            for fc in range(KC):
                sq = self.sq_rot()
                k.op("act", lambda E, sq=sq, xt=xt, fc=fc: E.activation(out=sq[:], in_=xt[:, fc, :], func=AF.Square), [xt], [sq])
                k.op("pe", lambda E, sq=sq, fc=fc: E.matmul(pss[0:1, :], self.ones_col[:, 0:1], sq[:], start=(fc == 0), stop=(fc == KC - 1)),
                     [sq, self.ones_col], [pss])
            rs = self.rs_rot()
            k.op("act", lambda E, rs=rs: E.activation(out=rs[:], in_=pss[0:1, :], func=AF.Sqrt, scale=1.0 / D, bias=self.eps_t[0:1, 0:1]),
                 [pss, self.eps_t], [rs])
            k.op("dve", lambda E, rs=rs: E.reciprocal(out=rs[:], in_=rs[:]), [rs], [rs])
            psb = self.ps_aux[2]
            k.op("pe", lambda E, rs=rs: E.matmul(psb[:, :], self.ones_row[0:1, :], rs[0:1, :], start=True, stop=True), [rs, self.ones_row], [psb])
            for fc in range(KC):
                tmp = self.sq_rot()
                k.op("dve", lambda E, tmp=tmp, xt=xt, fc=fc: E.tensor_tensor(out=tmp[:], in0=xt[:, fc, :], in1=psb[:, :], op=ALU.mult), [xt, psb], [tmp])
                k.op("act", lambda E, tmp=tmp, fc=fc, tt=tt: E.activation(out=hT[:, fc, tt * 512:(tt + 1) * 512], in_=tmp[:], func=AF.Identity,
                                                                    scale=a[:, fc:fc + 1], bias=sh[:, fc:fc + 1]), [tmp, a, self.modsb], [hT])
                if hf_cb is not None:
                    hf_cb(tt, fc, tmp, a, sh)
            if hf_cb is not None:
                hf_cb(tt, None, None, a, sh)
        k.pop()

    def layer(self, l):
        k, L = self.k, self.L
        if l == 0:
            self.eps_t = k.sb_main("eps", [1, 1], F32)
            k.op("dve", lambda E: E.memset(self.eps_t[:], EPS), [], [self.eps_t])
            self.hT = k.sb_main("hT", [128, KC, L], BF16)
            self.a_t = [k.sb_main("a0", [128, KC], F32), k.sb_main("a1", [128, KC], F32)]
        src = self.xT_in if l == 0 else self.xT
        self.norm_mod(l, 0, src, self.hT)
        hdump = k.dram(f"hT_dump{l}", [D, L], BF16)
        k.dma("sp", hdump.t.ap().rearrange("(fc p) t -> p fc t", p=128), self.hT[:], [self.hT], hdump)
        if l == 0:
            self.taps["hT"] = hdump
        if self.stop == "l0":
            return
        k.push()
        self.cg_f32 = self.rot("cgf", 2, [128, 4096], F32)
        self.cg_bf = self.rot("cgb", 2, [128, 4096], BF16)
        w_in = self.cast_gather(f"win{l}", self.w_in_sh, (D // NCORE) * D_IN, (D, D_IN), src_ap=self.w_in_sh.t.ap()[l])
        self.whd = self.cast_private(f"whead{l}", self.whead_in.t.ap()[l], self.whead_in, D * 1026)
        k.pop()
        self.w_in = w_in
        self.mixer_pre(l, w_in)
        self.attn_pre(l, w_in)
        k.push()
        self.cg_f32 = self.rot("cgf", 2, [128, 4096], F32)
        self.cg_bf = self.rot("cgb", 2, [128, 4096], BF16)
        ws = (D // NCORE) * D
        wpm = self.cast_gather(f"wpm{l}", self.wpm_sh, ws, (D, D), src_ap=self.wpm_sh.t.ap()[l])
        wpa = self.cast_gather(f"wpa{l}", self.wpa_sh, ws, (D, D), src_ap=self.wpa_sh.t.ap()[l])
        wo = self.cast_gather(f"wo{l}", self.wo_sh, ws, (D, D), src_ap=self.wo_sh.t.ap()[l])
        k.pop()
        ym_src = self.mlstm(l, w_in)
        if self.stop == "mlstm":
            self.tap("ym", ym_src)
            return
        self.attention(l)
        if self.stop == "attn":
            yd = k.dram(f"ya_dump{l}", [D, L], BF16)
            k.dma("sp", yd.t.ap().rearrange("(fc p) t -> p fc t", p=128), self.yaT[:], [self.yaT], yd)
            self.tap("yaT", yd)
            return
        self.mixer_post(l, ym_src, wpm, wpa, wo, src)
        if self.stop == "mix":
            self.tap("x1", self.xT)
            return
        self.moe(l, self.xT)

    def mixer_post(self, l, ym_src, wpm, wpa, wo, src):
        k, L, S = self.k, self.L, self.S
        NB = L // 128
        ds = bass.ds
        ymfull = k.dram(f"ymfull{l}", [NCORE * S, 256], BF16)
        k.allgather(ym_src, ymfull)
        k.push()
        ymT = k.sb("ymT", [128, KC, L], BF16)
        k.push()
        ymall = k.sb("ymall", [128, NH_M, NB, 256], BF16)
        yv = ymfull.t.ap().rearrange("(hd r j p) e -> p hd r j e", hd=NH_M, r=NCORE, p=128)
        for tb in range(NB):
            yt = ymall
            k.dma(("sp", "act")[tb % 2], ymall[:, :, tb, :], (lambda E, tb=tb: yv[:, :, ds(k.pid(E), 1), tb, :].rearrange("p hd o e -> p (hd o) e")), [ymfull], ymall)
            for f4 in range(KC // 4):
                for j in range(4):
                    fc = f4 * 4 + j
                    k.op("pe", lambda E, yt=yt, fc=fc, j=j, tb=tb: E.transpose(out=self.psb16[:, j * 128:(j + 1) * 128],
                                                                        in_=yt[:, fc // 2, tb, (fc % 2) * 128:(fc % 2 + 1) * 128], identity=self.identb[:]),
                         [yt, self.identb], [self.psb16], signal=(j == 3))
                for j in range(4):
                    fc = f4 * 4 + j
                    k.op("act", lambda E, fc=fc, j=j, tb=tb: E.activation(out=ymT[:, fc, tb * 128:(tb + 1) * 128], in_=self.psb16[:, j * 128:(j + 1) * 128],
                                                                         func=AF.Identity, scale=self.mn[:, l * KC + fc:l * KC + fc + 1]), [self.psb16, self.mn], [ymT])
        k.pop()
        mT = k.sb("mergedT", [128, KC, L], BF16)
        wrot = self.rot("wpj", 3, [128, KC, 512], BF16)
        grot = self.rot("gt", 4, [128, 512], BF16)
        trot = self.rot("tmg", 2, [128, 512], F32)
        view = lambda w: w.t.ap().rearrange("(r p) f -> (r p f)", p=128).rearrange("(kc p n) -> p kc n", p=128, n=D)
        for g0 in range(0, D, 512):
            wm, wa_ = wrot(), wrot()
            k.dma("sp", wm[:], view(wpm)[:, :, g0:g0 + 512], [wpm], wm)
            k.dma("act", wa_[:], view(wpa)[:, :, g0:g0 + 512], [wpa], wa_)
            for c4 in range(4):
                c = g0 // 128 + c4
                for tt in range(L // 512):
                    tsl = slice(tt * 512, (tt + 1) * 512)
                    pm, pa = self.next_ps(), self.next_ps()
                    for (pp, ww, act_) in ((pm, wm, ymT), (pa, wa_, self.yaT)):
                        for kc in range(KC):
                            k.op("pe", lambda E, pp=pp, ww=ww, act_=act_, kc=kc, c4=c4, tsl=tsl: E.matmul(
                                pp[:, :], ww[:, kc, c4 * 128:(c4 + 1) * 128], act_[:, kc, tsl], start=(kc == 0), stop=(kc == KC - 1)),
                                [ww, act_], [pp], signal=(kc == KC - 1))
                    gmt, gat = grot(), grot()
                    k.dma("sp", gmt[:], self.gmT.t.ap()[c * 128:(c + 1) * 128, tsl], [self.gmT], gmt)
                    k.dma("act", gat[:], self.gaT.t.ap()[c * 128:(c + 1) * 128, tsl], [self.gaT], gat)
                    t1, t2 = trot(), trot()
                    k.op("dve", lambda E, t1=t1, pm=pm, gmt=gmt: E.tensor_tensor(out=t1[:], in0=pm[:, :], in1=gmt[:], op=ALU.mult), [pm, gmt], [t1])
                    k.op("dve", lambda E, t2=t2, pa=pa, gat=gat: E.tensor_tensor(out=t2[:], in0=pa[:, :], in1=gat[:], op=ALU.mult), [pa, gat], [t2])
                    k.op("dve", lambda E, t1=t1, t2=t2, c=c, tsl=tsl: E.tensor_tensor(out=mT[:, c, tsl], in0=t1[:], in1=t2[:], op=ALU.add), [t1, t2], [mT])
        self.wg_rot = wrot
        xo = self.rot("xo2", 2, [128, 512], F32)
        g1 = self.modv(l, 2)
        xsrc = src.t.ap().rearrange("(fc p) t -> p fc t", p=128)
        xdst = self.xT.t.ap().rearrange("(fc p) t -> p fc t", p=128)

        def ep_res(c, tt, ps):
            xb = xo()
            k.dma(self.next_q(), xb[:], xsrc[:, c, tt * 512:(tt + 1) * 512], [src], xb)
            k.op("dve", lambda E, xb=xb, ps=ps, c=c: E.scalar_tensor_tensor(out=xb[:], in0=ps[:, :], scalar=g1[:, c:c + 1], in1=xb[:], op0=ALU.mult, op1=ALU.add),
                 [ps, xb, self.modsb], [xb])
            k.dma(self.next_q(), xdst[:, c, tt * 512:(tt + 1) * 512], xb[:], [xb], self.xT)
        self.gemmA(wo, 0, D, mT, ep_res)
        k.pop()

    def moe(self, l, src):
        k, L = self.k, self.L
        ES = (N_EXP // NCORE) * D * D_FF
        k.push()
        self.cg_f32 = self.rot("cgf", 2, [128, 4096], F32)
        self.cg_bf = self.rot("cgb", 2, [128, 4096], BF16)
        w1 = self.cast_gather(f"w1_{l}", self.w1_sh, ES, None, src_ap=self.w1_sh.t.ap()[l])
        w3 = self.cast_gather(f"w3_{l}", self.w3_sh, ES, None, src_ap=self.w3_sh.t.ap()[l])
        w2 = self.cast_gather(f"w2_{l}", self.w2_sh, ES, None, src_ap=self.w2_sh.t.ap()[l])
        k.pop()
        flat = lambda w: w.t.ap().rearrange("(r p) f -> (r p f)", p=128)
        w1v = flat(w1).rearrange("(e kc p n) -> e p kc n", p=128, kc=KC, n=D_FF)
        w3v = flat(w3).rearrange("(e kc p n) -> e p kc n", p=128, kc=KC, n=D_FF)
        w2v = flat(w2).rearrange("(e fc p n) -> e p fc n", p=128, fc=4, n=D)
        k.push()
        gateT = k.sb("gateT", [32, L], F32)
        wr = k.sb("wr", [128, KC, 36], F32)
        brb = k.sb("brb", [128, 36], F32)
        ident = k.sb("ident", [128, 128], F32)
        esel = k.sb("esel", [32, 32 * 128], F32)
        k.dma("sp", wr[:], self.wr_in.t.ap()[l].rearrange("p (kc n) -> p kc n", n=36), [self.wr_in], wr)
        k.dma("act", brb[:], self.br_in.t.ap()[l], [self.br_in], brb)
        k.dma("sp", ident[:], self.ident_in.t.ap(), [self.ident_in], ident)
        k.dma("act", esel[:], self.esel_in.t.ap(), [self.esel_in], esel)
        k.push()
        h2f = k.sb("h2f", [128, KC, 512], F32)
        sm = {n: k.sb("r_" + n, shp, F32) for n, shp in dict(lg=[128, 36], gmax=[128, 1], ngmax=[128, 1], ge=[128, 4], gsum=[128, 1],
                                                               oh=[128, 4], em=[128, 32], top8=[128, 8], nv1=[128, 1], sel=[128, 32], w=[128, 32],
                                                               s2=[128, 1], coef=[128, 1], gate=[128, 32]).items()}
        gate_dump = k.dram(f"gate_dump{l}", [L, 32], F32)
        psr_ = self.ps_aux[3]

        def hf_cb(tt, fc, tmp, a, sh):
            if fc is not None:
                k.op("act", lambda E: E.activation(out=h2f[:, fc, :], in_=tmp[:], func=AF.Identity, scale=a[:, fc:fc + 1], bias=sh[:, fc:fc + 1]),
                     [tmp, a, self.modsb], [h2f])
                return
            for tb in range(4):
                t0 = tt * 512 + tb * 128
                for kc in range(KC):
                    k.op("pe", lambda E, kc=kc, tb=tb: E.matmul(psr_[:, 0:36], h2f[:, kc, tb * 128:(tb + 1) * 128], wr[:, kc, :],
                                                              start=(kc == 0), stop=(kc == KC - 1)), [h2f, wr], [psr_], signal=(kc == KC - 1))
                m = sm
                V = lambda f, r, w: k.op("dve", f, r, w)
                V(lambda E: E.tensor_tensor(out=m["lg"][:], in0=psr_[:, 0:36], in1=brb[:], op=ALU.add), [psr_, brb], [m["lg"]])
                V(lambda E: E.reduce_max(out=m["gmax"][:], in_=m["lg"][:, 0:4], axis=AX.X), [m["lg"]], [m["gmax"]])
                V(lambda E: E.tensor_scalar(out=m["ngmax"][:], in0=m["gmax"][:], scalar1=-1.0, scalar2=None, op0=ALU.mult), [m["gmax"]], [m["ngmax"]])
                k.op("act", lambda E: E.activation(out=m["ge"][:], in_=m["lg"][:, 0:4], func=AF.Exp, bias=m["ngmax"][:, 0:1], scale=1.0),
                     [m["lg"], m["ngmax"]], [m["ge"]])
                V(lambda E: E.reduce_sum(out=m["gsum"][:], in_=m["ge"][:], axis=AX.X), [m["ge"]], [m["gsum"]])
                V(lambda E: E.tensor_scalar(out=m["oh"][:], in0=m["lg"][:, 0:4], scalar1=m["gmax"][:, 0:1], scalar2=None, op0=ALU.is_ge), [m["lg"], m["gmax"]], [m["oh"]])
                V(lambda E: E.tensor_scalar(out=m["oh"][:], in0=m["oh"][:], scalar1=-1.0, scalar2=100.0, op0=ALU.add, op1=ALU.mult), [m["oh"]], [m["oh"]])
                for g in range(4):
                    V(lambda E, g=g: E.tensor_scalar(out=m["em"][:, g * 8:(g + 1) * 8], in0=m["lg"][:, 4 + g * 8:12 + g * 8], scalar1=m["oh"][:, g:g + 1],
                                                     scalar2=None, op0=ALU.add), [m["lg"], m["oh"]], [m["em"]])
                V(lambda E: E.max(out=m["top8"][:], in_=m["em"][:]), [m["em"]], [m["top8"]])
                V(lambda E: E.tensor_scalar(out=m["nv1"][:], in0=m["top8"][:, 0:1], scalar1=-1.0, scalar2=None, op0=ALU.mult), [m["top8"]], [m["nv1"]])
                V(lambda E: E.tensor_scalar(out=m["sel"][:], in0=m["em"][:], scalar1=m["top8"][:, 1:2], scalar2=None, op0=ALU.is_ge), [m["em"], m["top8"]], [m["sel"]])
                k.op("act", lambda E: E.activation(out=m["w"][:], in_=m["em"][:], func=AF.Exp, bias=m["nv1"][:, 0:1], scale=1.0), [m["em"], m["nv1"]], [m["w"]])
                k.op("act", lambda E: E.activation(out=m["s2"][:], in_=m["top8"][:, 1:2], func=AF.Exp, bias=m["nv1"][:, 0:1], scale=1.0), [m["top8"], m["nv1"]], [m["s2"]])
                V(lambda E: E.scalar_tensor_tensor(out=m["coef"][:], in0=m["s2"][:], scalar=1.0, in1=m["gsum"][:], op0=ALU.add, op1=ALU.mult),
                  [m["s2"], m["gsum"]], [m["coef"]])
                V(lambda E: E.reciprocal(out=m["coef"][:], in_=m["coef"][:]), [m["coef"]], [m["coef"]])
                V(lambda E: E.scalar_tensor_tensor(out=m["gate"][:], in0=m["w"][:], scalar=m["coef"][:, 0:1], in1=m["sel"][:], op0=ALU.mult, op1=ALU.mult),
                  [m["w"], m["coef"], m["sel"]], [m["gate"]])
                k.dma("sp", gate_dump.t.ap()[t0:t0 + 128, :], m["gate"][:], [m["gate"]], gate_dump)
                k.op("pe", lambda E: E.transpose(out=psr_[0:32, 128:256], in_=m["gate"][:], identity=ident[:]), [m["gate"], ident], [psr_])
                V(lambda E, t0=t0: E.tensor_copy(out=gateT[:, t0:t0 + 128], in_=psr_[0:32, 128:256]), [psr_], [gateT])

        self.norm_mod(l, 1, src, self.hT, hf_cb=hf_cb)
        k.pop()
        self.tap("gate", gate_dump)
        if self.stop == "router":
            k.pop()
            return
        w1r = self.rot("w1e", 2, [128, KC, D_FF], BF16)
        w3r = self.rot("w3e", 1, [128, KC, D_FF], BF16)
        w2r = self.rot("w2e", 1, [128, 4, D], BF16)
        acc = k.sb("acc", [128, KC, 512], F32)
        sl = self.rot("silu", 2, [128, 512], F32)
        ugr = [self.rot(f"ug{i}", 2, [128, 512], BF16) for i in range(4)]
        xo = self.rot("xo", 2, [128, 512], F32)
        g2 = self.modv(l, 5)
        psg = self.ps_aux[3]
        xsrc = src.t.ap().rearrange("(fc p) t -> p fc t", p=128)
        xdst = self.xT.t.ap().rearrange("(fc p) t -> p fc t", p=128)
        for tt in range(L // 512):
            k.op("dve", lambda E: E.memset(acc[:], 0.0), [], [acc])
            for e in range(N_EXP):
                w1e, w3e, w2e = w1r(), w3r(), w2r()
                k.dma("sp", w1e[:], w1v[e], [w1], w1e)
                k.dma("act", w3e[:], w3v[e], [w3], w3e)
                k.dma("sp", w2e[:], w2v[e], [w2], w2e)
                k.op("pe", lambda E, e=e, tt=tt: E.matmul(psg[:, :], esel[:, e * 128:(e + 1) * 128], gateT[:, tt * 512:(tt + 1) * 512], start=True, stop=True),
                     [esel, gateT], [psg])
                ugs = []
                for fc in range(4):
                    pa, pb = self.next_ps(), self.next_ps()
                    for (pp, ww) in ((pa, w1e), (pb, w3e)):
                        for kc in range(KC):
                            k.op("pe", lambda E, pp=pp, ww=ww, kc=kc, fc=fc, tt=tt: E.matmul(
                                pp[:, :], ww[:, kc, fc * 128:(fc + 1) * 128], self.hT[:, kc, tt * 512:(tt + 1) * 512],
                                start=(kc == 0), stop=(kc == KC - 1)), [ww, self.hT], [pp], signal=(kc == KC - 1))
                    sb_ = sl()
                    ug = ugr[fc]()
                    k.op("act", lambda E, sb_=sb_, pa=pa: E.activation(out=sb_[:], in_=pa[:, :], func=AF.Silu), [pa], [sb_])
                    k.op("dve", lambda E, sb_=sb_, pb=pb: E.tensor_tensor(out=sb_[:], in0=sb_[:], in1=pb[:, :], op=ALU.mult), [sb_, pb], [sb_])
                    k.op("dve", lambda E, sb_=sb_, ug=ug: E.tensor_tensor(out=ug[:], in0=sb_[:], in1=psg[:, :], op=ALU.mult), [sb_, psg], [ug])
                    ugs.append(ug)
                for oc in range(KC):
                    py = self.next_ps()
                    for fc in range(4):
                        k.op("pe", lambda E, py=py, fc=fc, oc=oc, w2e=w2e, ug=ugs[fc]: E.matmul(
                            py[:, :], w2e[:, fc, oc * 128:(oc + 1) * 128], ug[:], start=(fc == 0), stop=(fc == 3)),
                            [w2e, ugs[fc]], [py], signal=(fc == 3))
                    k.op("dve", lambda E, py=py, oc=oc: E.tensor_tensor(out=acc[:, oc, :], in0=acc[:, oc, :], in1=py[:, :], op=ALU.add), [acc, py], [acc])
            for oc in range(KC):
                xb = xo()
                k.dma(self.next_q(), xb[:], xsrc[:, oc, tt * 512:(tt + 1) * 512], [src], xb)
                k.op("dve", lambda E, xb=xb, oc=oc: E.scalar_tensor_tensor(out=xb[:], in0=acc[:, oc, :], scalar=g2[:, oc:oc + 1], in1=xb[:],
                                                                          op0=ALU.mult, op1=ALU.add), [acc, xb, self.modsb], [xb])
                k.dma(self.next_q(), xdst[:, oc, tt * 512:(tt + 1) * 512], xb[:], [xb], self.xT)
        k.pop()
        self.tap("x1", self.xT)

    def build_bias_tables(self):
        k = self.k
        self.expT = k.sb("expT", [128, 2, 16, 128], BF16)
        self.kpos = k.sb("kpos", [128, 512], F32)
        self.qpos = k.sb("qpos", [128, self.L // 128], F32)
        self.qn = k.sb("qn", [128, self.depth], F32)
        self.kn = k.sb("kn", [128, self.depth], F32)
        self.mn = k.sb("mn", [128, self.depth * KC], F32)
        k.dma("sp", self.kpos[:], self.kpos_in.t.ap(), [self.kpos_in], self.kpos)
        k.dma("act", self.qpos[:], self.qpos_in.t.ap(), [self.qpos_in], self.qpos)
        k.dma("sp", self.qn[:], self.qn_in.t.ap(), [self.qn_in], self.qn)
        k.dma("act", self.kn[:], self.kn_in.t.ap(), [self.kn_in], self.kn)
        k.dma("sp", self.mn[:], self.mn_in.t.ap(), [self.mn_in], self.mn)
        k.push()
        rb = k.sb("relb", [32, 16], F32)
        es31 = k.sb("es31", [32, 128], F32)
        k.dma("sp", rb[:], self.relb_in.t.ap(), [self.relb_in], rb)
        k.dma("act", es31[:], self.esel_in.t.ap()[:, 31 * 128:32 * 128], [self.esel_in], es31)
        c31 = k.sb("c31", [128, 16], F32)
        pst = self.ps_aux[0]
        k.op("pe", lambda E: E.matmul(pst[:, 0:16], es31[:], rb[:], start=True, stop=True), [es31, rb], [pst])
        k.op("dve", lambda E: E.tensor_copy(out=c31[:], in_=pst[:, 0:16]), [pst], [c31])
        Tb = k.sb("Tb", [128, 128, 16], F32)
        oh_rot = self.rot("bkt", 2, [32, 32 * 128], F32)
        for di in range(2):
            for qp in range(4):
                oh = oh_rot()
                k.dma(self.next_q(), oh[:], self.bkt_in.t.ap()[di][:, qp * 4096:(qp + 1) * 4096], [self.bkt_in], oh)
                ps = self.next_ps()
                for qq in range(32):
                    k.op("pe", lambda E, ps=ps, oh=oh, qq=qq: E.matmul(ps[:, qq * 16:(qq + 1) * 16], oh[:, qq * 128:(qq + 1) * 128], rb[:], start=True, stop=True),
                         [oh, rb], [ps], signal=(qq == 31))
                k.op("dve", lambda E, ps=ps, qp=qp: E.tensor_tensor(out=Tb[:, qp * 32:(qp + 1) * 32, :], in0=ps[:, :].rearrange("p (q h) -> p q h", h=16),
                                                                     in1=c31[:].unsqueeze(1).broadcast_to([128, 32, 16]), op=ALU.subtract), [ps, c31], [Tb])
            k.op("act", lambda E: E.activation(out=Tb[:], in_=Tb[:], func=AF.Exp), [Tb], [Tb])
            k.op("dve", lambda E, di=di: E.tensor_scalar(out=self.expT[:, di], in0=Tb[:].rearrange("p q h -> p h q"), scalar1=-1.0, scalar2=None, op0=ALU.add),
                 [Tb], [self.expT])
        k.pop()

    def mixer_pre(self, l, w_in):
        k, L = self.k, self.L
        k.push()
        hsrc = k.dram(f"hsrc{l}", [D, L], BF16)
        self.hfull = k.dram(f"hfull{l}", [NCORE * D, L], BF16)
        k.dma("act", hsrc.t.ap().rearrange("(fc p) t -> p fc t", p=128), self.hT[:], [self.hT], hsrc)
        k.allgather(hsrc, self.hfull)
        k.pop()

    def mlstm(self, l, w_in):
        k, L, S = self.k, self.L, self.S
        NCH = S // 128
        CPR = L // 128
        ds = bass.ds
        k.push()
        wv = w_in.t.ap().rearrange("(r p) f -> (r p f)", p=128).rearrange("(kc p n) -> p kc n", p=128, n=D_IN)
        whv = self.whd.t.ap().rearrange("p f -> (p f)").rearrange("(kc p n) -> p kc n", p=128, n=1026)
        wall = k.sb("wh_all", [128, KC, 4, 256], BF16)
        for sg in range(4):
            k.dma(self.next_q(), wall[:, :, sg, :], whv[:, :, sg * 256:(sg + 1) * 256], [self.whd], wall)
        wgate = k.sb("wgate", [128, KC, 2], BF16)
        k.dma("sp", wgate[:], whv[:, :, 1024:1026], [self.whd], wgate, allow_slow_non_contiguous=True)
        wts = {nm: (lambda kc, sl, i=i: wall[:, kc, i, sl]) for i, nm in enumerate(("q", "k", "v", "o"))}
        cwall = k.sb("cw_all", [128, 2, 2, 4], F32)
        k.dma("act", cwall[:].rearrange("p a b c -> p (a b c)"), self.convh_in.t.ap()[l], [self.convh_in], cwall)
        cw = {"q": cwall[:, 0], "k": cwall[:, 1]}
        cwb = cwall
        bg2 = k.sb("bg2", [NCH, 2], F32)
        k.dma("sp", bg2[:], self.bgh_in.t.ap()[l][0:NCH, :], [self.bgh_in], bg2)
        G = lambda nm, shp=(NCH, 128): k.sb("g_" + nm, list(shp), F32)
        gT = k.dram(f"gT{l}", [2, S], F32)
        hvv = self.hfull.t.ap().rearrange("(r kc p) t -> r p kc t", p=128, kc=KC)
        hxg = self.rot("hxg", 2, [128, KC, 512], BF16)
        gsb2 = self.rot("gsb2", 2, [2, 512], F32)
        for r in range(NCORE):
            for tt in range(L // 512):
                hx = hxg()
                k.dma(self.next_q(), hx[:], hvv[r][:, :, tt * 512:(tt + 1) * 512], [self.hfull], hx)
                ps = self.next_ps()
                for kc in range(KC):
                    k.op("pe", lambda E, ps=ps, kc=kc, hx=hx: E.matmul(ps[0:2, :], wgate[:, kc, :], hx[:, kc, :], start=(kc == 0), stop=(kc == KC - 1)),
                         [wgate, hx], [ps], signal=(kc == KC - 1))
                gs_ = gsb2()
                k.op("act", lambda E, ps=ps, gs_=gs_: E.copy(out=gs_[:], in_=ps[0:2, :]), [ps], [gs_])
                k.dma(self.next_q(), gT.t.ap()[:, r * L + tt * 512:r * L + (tt + 1) * 512], gs_[:], [gs_], gT)
        gboth = k.sb("g_both", [NCH, 2, 128], F32)
        k.dma("sp", gboth[:], gT.t.ap().rearrange("g (c l) -> c g l", l=128), [gT], gboth)
        ig, fp_ = G("ig"), G("fp")
        V = lambda f, r, w: k.op("dve", f, r, w)
        A = lambda f, r, w: k.op("act", f, r, w)
        ones = G("ones"); zeros = G("zeros")
        V(lambda E: E.memset(ones[:], 1.0), [], [ones])
        V(lambda E: E.memset(zeros[:], 0.0), [], [zeros])
        V(lambda E: E.tensor_scalar(out=ig[:], in0=gboth[:, 0, :], scalar1=bg2[:, 0:1], scalar2=None, op0=ALU.add), [gboth, bg2], [ig])
        V(lambda E: E.tensor_scalar(out=fp_[:], in0=gboth[:, 1, :], scalar1=bg2[:, 1:2], scalar2=None, op0=ALU.add), [gboth, bg2], [fp_])
        lf = G("lf"); b = G("b"); u = G("u"); cm = G("cm"); a_ = G("a")
        A(lambda E: E.activation(out=lf[:], in_=fp_[:], func=AF.Exp, scale=-1.0), [fp_], [lf])
        A(lambda E: E.activation(out=lf[:], in_=lf[:], func=AF.Ln, bias=ones[:, 0:1], scale=1.0), [lf, ones], [lf])
        V(lambda E: E.tensor_scalar(out=lf[:], in0=lf[:], scalar1=-1.0, scalar2=None, op0=ALU.mult), [lf], [lf])
        V(lambda E: E.tensor_tensor_scan(out=b[:], data0=ones[:], data1=lf[:], initial=0.0, op0=ALU.mult, op1=ALU.add), [ones, lf], [b])
        V(lambda E: E.tensor_tensor(out=u[:], in0=ig[:], in1=b[:], op=ALU.subtract), [ig, b], [u])
        V(lambda E: E.tensor_tensor_scan(out=cm[:], data0=zeros[:], data1=u[:], initial=-1e30, op0=ALU.add, op1=ALU.max), [zeros, u], [cm])
        m_intra = cm
        V(lambda E: E.tensor_tensor(out=m_intra[:], in0=cm[:], in1=b[:], op=ALU.add), [cm, b], [m_intra])
        V(lambda E: E.tensor_scalar(out=a_[:], in0=u[:], scalar1=b[:, 127:128], scalar2=None, op0=ALU.add), [u, b], [a_])
        mloc = G("mloc", (NCH, 1)); nml = G("nml", (NCH, 1))
        V(lambda E: E.reduce_max(out=mloc[:], in_=a_[:], axis=AX.X), [a_], [mloc])
        V(lambda E: E.tensor_scalar(out=nml[:], in0=mloc[:], scalar1=-1.0, scalar2=None, op0=ALU.mult), [mloc], [nml])
        wloc = a_
        A(lambda E: E.activation(out=wloc[:], in_=a_[:], func=AF.Exp, bias=nml[:, 0:1], scale=1.0), [a_, nml], [wloc])
        pst = self.ps_aux[0]
        rows = k.sb("rows", [1, 6 * NCH], F32)
        R = lambda i: rows[0:1, i * NCH:(i + 1) * NCH]
        k.op("pe", lambda E: E.transpose(out=pst[0:1, 0:NCH], in_=mloc[:, 0:1], identity=self.ident[0:NCH, 0:NCH]), [mloc, self.ident], [pst])
        k.op("pe", lambda E: E.transpose(out=pst[0:1, NCH:2 * NCH], in_=b[:, 127:128], identity=self.ident[0:NCH, 0:NCH]), [b, self.ident], [pst])
        V(lambda E: E.tensor_copy(out=rows[0:1, 0:2 * NCH], in_=pst[0:1, 0:2 * NCH]), [pst], [rows])
        V(lambda E: E.tensor_tensor_scan(out=R(2), data0=R(1), data1=R(0), initial=0.0, op0=ALU.add, op1=ALU.max), [rows], [rows])
        V(lambda E: E.memset(rows[0:1, 3 * NCH:3 * NCH + 1], 0.0), [], [rows])
        V(lambda E: E.tensor_copy(out=rows[0:1, 3 * NCH + 1:4 * NCH], in_=rows[0:1, 2 * NCH:3 * NCH - 1]), [rows], [rows])
        V(lambda E: E.tensor_tensor(out=R(4), in0=R(1), in1=R(3), op=ALU.add), [rows], [rows])
        V(lambda E: E.tensor_tensor(out=R(4), in0=R(4), in1=R(2), op=ALU.subtract), [rows], [rows])
        V(lambda E: E.tensor_tensor(out=R(5), in0=R(0), in1=R(2), op=ALU.subtract), [rows], [rows])
        A(lambda E: E.activation(out=rows[0:1, 4 * NCH:6 * NCH], in_=rows[0:1, 4 * NCH:6 * NCH], func=AF.Exp), [rows], [rows])
        bc = k.sb("bc", [128, 2 * NCH], F32)
        k.op("pe", lambda E: E.matmul(pst[:, 0:2 * NCH], self.ones_row[0:1, :], rows[0:1, 4 * NCH:6 * NCH], start=True, stop=True), [rows, self.ones_row], [pst])
        V(lambda E: E.tensor_copy(out=bc[:], in_=pst[:, 0:2 * NCH]), [pst], [bc])
        mprev = G("mprev", (NCH, 1))
        k.op("pe", lambda E: E.transpose(out=pst[0:NCH, 0:1], in_=R(3), identity=self.ident[0:1, 0:1]), [rows, self.ident], [pst])
        V(lambda E: E.tensor_copy(out=mprev[:], in_=pst[0:NCH, 0:1]), [pst], [mprev])
        inter = G("inter"); mt = G("mt")
        V(lambda E: E.tensor_scalar(out=inter[:], in0=b[:], scalar1=mprev[:, 0:1], scalar2=None, op0=ALU.add), [b, mprev], [inter])
        V(lambda E: E.tensor_tensor(out=mt[:], in0=inter[:], in1=m_intra[:], op=ALU.max), [inter, m_intra], [mt])
        V(lambda E: E.tensor_tensor(out=inter[:], in0=inter[:], in1=mt[:], op=ALU.subtract), [inter, mt], [inter])
        V(lambda E: E.tensor_tensor(out=b[:], in0=b[:], in1=mt[:], op=ALU.subtract), [b, mt], [b])
        A(lambda E: E.activation(out=u[:], in_=u[:], func=AF.Exp), [u], [u])
        A(lambda E: E.activation(out=b[:], in_=b[:], func=AF.Exp), [b], [b])
        A(lambda E: E.activation(out=inter[:], in_=inter[:], func=AF.Exp), [inter], [inter])
        A(lambda E: E.activation(out=mt[:], in_=mt[:], func=AF.Exp, scale=-1.0), [mt], [mt])
        cols = {}
        for nm, src_ in (("EU", u), ("W", wloc), ("ER", b), ("S", inter), ("EM", mt)):
            cols[nm] = k.sb("col" + nm, [128, NCH], F32)
            k.op("pe", lambda E, src_=src_: E.transpose(out=pst[:, 0:NCH], in_=src_[:], identity=self.ident[0:NCH, 0:NCH]), [src_, self.ident], [pst])
            V(lambda E, nm=nm: E.tensor_copy(out=cols[nm][:], in_=pst[:, 0:NCH]), [pst], [cols[nm]])
        ym_src = k.dram(f"ym_src{l}", [S, 256], BF16)
        hx_rot = hxg
        raw = {nm: [k.sb(f"raw{nm}{i}", [128, 2, 515], F32) for i in range(2)] for nm in ("q", "k")}
        for nm in ("q", "k"):
            V(lambda E, nm=nm: E.memset(raw[nm][0][:, :, 0:3], 0.0), [], [raw[nm][0]])
        cacc = k.sb("cacc", [128, 512], F32)
        qT = k.sb("qT", [128, 2, 512], BF16)
        kT = k.sb("kT", [128, 2, 512], BF16)
        vaug = [k.sb(f"vaug{i}", [128, 257], BF16) for i in range(2)]
        for i in range(2):
            V(lambda E, i=i: E.memset(vaug[i][:, 256:257], 1.0), [], [vaug[i]])
        sgo = k.sb("sgo", [128, 256], BF16)
        kw = k.sb("kw", [128, 256], BF16)
        Wp = k.sb("Wp", [128, 128], BF16)
        t1 = k.sb("t1", [128, 257], F32)
        nd = k.sb("nd", [128, 257], F32)
        den = k.sb("den", [128, 1], F32)
        hm = k.sb("hm", [128, 256], F32)
        junk = k.sb("junk", [128, 256], F32)
        ss = k.sb("ss", [128, 1], F32)
        ymt = self.rot("ymt", 2, [128, 256], BF16)
        C = k.sb("Cst", [128, 2, 257], F32)
        Cbf = k.sb("Cbf", [128, 2, 257], BF16)
        ctmp = k.sb("ctmp", [128, 257], F32)
        V(lambda E: E.memset(C[:], 0.0), [], [C])
        V(lambda E: E.memset(Cbf[:], 0.0), [], [Cbf])
        hv = self.hfull.t.ap().rearrange("(r kc p) t -> r p kc t", p=128, kc=KC)
        ti = 0
        for r in range(NCORE):
            for tt in range(L // 512):
                hx = hx_rot()
                k.dma(self.next_q(), hx[:], hv[r][:, :, tt * 512:(tt + 1) * 512], [self.hfull], hx)
                cur, nxt = ti % 2, (ti + 1) % 2
                ti += 1
                for nm, dst in (("q", qT), ("k", kT)):
                    rw, rn = raw[nm][cur], raw[nm][nxt]
                    for half in range(2):
                        ps = self.next_ps()
                        for kc in range(KC):
                            k.op("pe", lambda E, ps=ps, kc=kc, half=half, nm=nm, hx=hx: E.matmul(
                                ps[:, :], wts[nm](kc, slice(half * 128, (half + 1) * 128)), hx[:, kc, :], start=(kc == 0), stop=(kc == KC - 1)),
                                [wall, hx], [ps], signal=(kc == KC - 1))
                        A(lambda E, ps=ps, rw=rw, half=half: E.copy(out=rw[:, half, 3:515], in_=ps[:, :]), [ps], [rw])
                    V(lambda E, rw=rw, rn=rn: E.tensor_copy(out=rn[:, :, 0:3], in_=rw[:, :, 512:515]), [rw], [rn])
                    for half in range(2):
                        V(lambda E, rw=rw, half=half, nm=nm: E.tensor_scalar(out=cacc[:], in0=rw[:, half, 0:512], scalar1=cw[nm][:, half, 0:1], scalar2=None,
                                                                         op0=ALU.mult), [rw, cwb], [cacc])
                        for j in range(1, 4):
                            V(lambda E, rw=rw, half=half, nm=nm, j=j: E.scalar_tensor_tensor(out=cacc[:], in0=rw[:, half, j:j + 512], scalar=cw[nm][:, half, j:j + 1],
                                                                                          in1=cacc[:], op0=ALU.mult, op1=ALU.add), [rw, cwb, cacc], [cacc])
                        A(lambda E, dst=dst, half=half: E.activation(out=dst[:, half, :], in_=cacc[:], func=AF.Silu), [cacc], [dst])
                    if nm == "k":
                        V(lambda E: E.tensor_scalar(out=kT[:], in0=kT[:], scalar1=1.0 / 16.0, scalar2=None, op0=ALU.mult), [kT], [kT])
                for j4 in range(4):
                    c = r * CPR + tt * 4 + j4
                    ts_ = slice(j4 * 128, (j4 + 1) * 128)
                    va = vaug[c % 2]
                    ps = self.next_ps()
                    for kc in range(KC):
                        k.op("pe", lambda E, ps=ps, kc=kc, hx=hx, ts_=ts_: E.matmul(ps[:, 0:256], hx[:, kc, ts_], wts["v"](kc, slice(0, 256)), start=(kc == 0), stop=(kc == KC - 1)),
                             [hx, wall], [ps], signal=(kc == KC - 1))
                    A(lambda E, ps=ps, va=va: E.copy(out=va[:, 0:256], in_=ps[:, 0:256]), [ps], [va])
                    ps = self.next_ps()
                    for kc in range(KC):
                        k.op("pe", lambda E, ps=ps, kc=kc, hx=hx, ts_=ts_: E.matmul(ps[:, 0:256], hx[:, kc, ts_], wts["o"](kc, slice(0, 256)), start=(kc == 0), stop=(kc == KC - 1)),
                             [hx, wall], [ps], signal=(kc == KC - 1))
                    A(lambda E, ps=ps: E.activation(out=sgo[:], in_=ps[:, 0:256], func=AF.Sigmoid), [ps], [sgo])
                    for half in range(2):
                        k.op("pe", lambda E, half=half, ts_=ts_: E.transpose(out=self.psb16[:, half * 128:(half + 1) * 128], in_=kT[:, half, ts_], identity=self.identb[:]),
                             [kT, self.identb], [self.psb16])
                    V(lambda E, c=c: E.tensor_scalar(out=kw[:], in0=self.psb16[:, 0:256], scalar1=cols["W"][:, c:c + 1], scalar2=None, op0=ALU.mult),
                      [self.psb16, cols["W"]], [kw])
                    ps = self.next_ps()
                    for half in range(2):
                        k.op("pe", lambda E, ps=ps, half=half, ts_=ts_: E.matmul(ps[:, 0:128], kT[:, half, ts_], qT[:, half, ts_], start=(half == 0), stop=(half == 1)),
                             [kT, qT], [ps], signal=(half == 1))
                    V(lambda E, ps=ps, c=c: E.scalar_tensor_tensor(out=Wp[:], in0=ps[:, 0:128], scalar=cols["EU"][:, c:c + 1], in1=self.causalT[:], op0=ALU.mult, op1=ALU.mult),
                      [ps, cols["EU"], self.causalT], [Wp])
                    psI = self.next_ps()
                    k.op("pe", lambda E, psI=psI, va=va: E.matmul(psI[:, 0:257], Wp[:], va[:], start=True, stop=True), [Wp, va], [psI])
                    V(lambda E, psI=psI, c=c: E.tensor_scalar(out=t1[:], in0=psI[:, 0:257], scalar1=cols["ER"][:, c:c + 1], scalar2=None, op0=ALU.mult), [psI, cols["ER"]], [t1])
                    psC = self.next_ps()
                    for half in range(2):
                        k.op("pe", lambda E, psC=psC, half=half, ts_=ts_: E.matmul(psC[:, 0:257], qT[:, half, ts_], Cbf[:, half, :], start=(half == 0), stop=(half == 1)),
                             [qT, Cbf], [psC], signal=(half == 1))
                    V(lambda E, psC=psC, c=c: E.scalar_tensor_tensor(out=nd[:], in0=psC[:, 0:257], scalar=cols["S"][:, c:c + 1], in1=t1[:], op0=ALU.mult, op1=ALU.add),
                      [psC, cols["S"], t1], [nd])
                    A(lambda E: E.activation(out=den[:], in_=nd[:, 256:257], func=AF.Abs), [nd], [den])
                    V(lambda E, c=c: E.tensor_scalar(out=den[:], in0=den[:], scalar1=cols["EM"][:, c:c + 1], scalar2=None, op0=ALU.max), [den, cols["EM"]], [den])
                    V(lambda E: E.reciprocal(out=den[:], in_=den[:]), [den], [den])
                    V(lambda E: E.tensor_scalar(out=hm[:], in0=nd[:, 0:256], scalar1=den[:, 0:1], scalar2=None, op0=ALU.mult), [nd, den], [hm])
                    V(lambda E: E.tensor_tensor(out=junk[:], in0=hm[:], in1=hm[:], op=ALU.mult), [hm], [junk])
                    V(lambda E: E.reduce_sum(out=ss[:], in_=junk[:], axis=AX.X), [junk], [ss])
                    A(lambda E: E.activation(out=ss[:], in_=ss[:], func=AF.Sqrt, scale=1.0 / 256.0, bias=self.eps128[:, 0:1]), [ss, self.eps128], [ss])
                    V(lambda E: E.reciprocal(out=ss[:], in_=ss[:]), [ss], [ss])
                    ym = ymt()
                    V(lambda E, ym=ym: E.scalar_tensor_tensor(out=ym[:], in0=hm[:], scalar=ss[:, 0:1], in1=sgo[:], op0=ALU.mult, op1=ALU.mult), [hm, ss, sgo], [ym])
                    k.dma(self.next_q(), ym_src.t.ap()[c * 128:(c + 1) * 128, :], ym[:], [ym], ym_src)
                    for half in range(2):
                        psL = self.next_ps()
                        k.op("pe", lambda E, psL=psL, half=half, va=va: E.matmul(psL[:, 0:257], kw[:, half * 128:(half + 1) * 128], va[:], start=True, stop=True), [kw, va], [psL])
                        V(lambda E, psL=psL, c=c: E.tensor_scalar(out=ctmp[:], in0=psL[:, 0:257], scalar1=bc[:, NCH + c:NCH + c + 1], scalar2=None, op0=ALU.mult), [psL, bc], [ctmp])
                        V(lambda E, half=half, c=c: E.scalar_tensor_tensor(out=C[:, half, :], in0=C[:, half, :], scalar=bc[:, c:c + 1], in1=ctmp[:], op0=ALU.mult, op1=ALU.add),
                          [C, bc, ctmp], [C])
                    A(lambda E: E.copy(out=Cbf[:], in_=C[:]), [C], [Cbf])
        k.pop()
        return ym_src

    def rms_rows(self, tmp, gain_ap, out_ap, nrm_d):
        k = self.k
        sq = self.sq2_rot()
        k.op("act", lambda E: E.activation(out=sq[:], in_=tmp[:], func=AF.Square), [tmp], [sq])
        pss = self.ps_aux[1]
        k.op("pe", lambda E: E.matmul(pss[0:1, :], self.ones_col[:, 0:1], sq[:], start=True, stop=True), [sq, self.ones_col], [pss])
        rs = self.rs2_rot()
        k.op("act", lambda E: E.activation(out=rs[:], in_=pss[0:1, :], func=AF.Sqrt, scale=1.0 / nrm_d, bias=self.eps_t[0:1, 0:1]), [pss, self.eps_t], [rs])
        k.op("dve", lambda E: E.reciprocal(out=rs[:], in_=rs[:]), [rs], [rs])
        psb = self.ps_aux[2]
        k.op("pe", lambda E: E.matmul(psb[:, :], self.ones_row[0:1, :], rs[0:1, :], start=True, stop=True), [rs, self.ones_row], [psb])
        k.op("dve", lambda E: E.tensor_tensor(out=sq[:], in0=tmp[:], in1=psb[:, :], op=ALU.mult), [tmp, psb], [sq])
        return sq

    def attn_pre(self, l, w_in):
        k, L = self.k, self.L
        NB = L // 128
        self.qaT = k.dram(f"qaT{l}", [128, NH_A, L], BF16)
        if l == 0:
            self.qiT = k.sb_main("qiT", [128, NH_I // 2, L], BF16)
            self.wi_sb = k.sb_main("wi", [128, NB, 16], F32)
        self.gmT = k.dram(f"gmT{l}", [D, L], BF16)
        self.gaT = k.dram(f"gaT{l}", [D, L], BF16)
        k.push()
        self.wg_rot = self.rot("wg", 2, [128, KC, 512], BF16)
        self.ev_rot = self.rot("ev", 3, [128, 512], BF16)
        self.sq2_rot = self.rot("sq2", 3, [128, 512], F32)
        self.rs2_rot = self.rot("rs2", 2, [1, 512], F32)
        tmp_rot = self.rot("tmpf", 2, [128, 512], F32)
        kaT = k.sb("kaT", [128, NKV, L], BF16)
        kiT = k.sb("kiT", [64, L], BF16)

        def ep_sig(dst):
            def ep(c, tt, ps):
                ev = self.ev_rot()
                k.op("act", lambda E, ev=ev, ps=ps: E.activation(out=ev[:], in_=ps[:, :], func=AF.Sigmoid), [ps], [ev])
                k.dma(self.next_q(), dst.t.ap()[c * 128:(c + 1) * 128, tt * 512:(tt + 1) * 512], ev[:], [ev], dst)
            return ep
        self.gemmA(w_in, OFF["gm"], 2048, self.hT, ep_sig(self.gmT))
        self.gemmA(w_in, OFF["ga"], 2048, self.hT, ep_sig(self.gaT))

        def ep_norm(dst, gain):
            def ep(c, tt, ps):
                tmp = tmp_rot()
                k.op("act", lambda E, tmp=tmp, ps=ps: E.copy(out=tmp[:], in_=ps[:, :]), [ps], [tmp])
                r = self.rms_rows(tmp, None, None, 128.0)
                if dst.dram:
                    ev = self.ev_rot()
                    k.op("act", lambda E, r=r, ev=ev: E.activation(out=ev[:], in_=r[:], func=AF.Identity, scale=gain[:, l:l + 1]), [r, gain], [ev])
                    k.dma(self.next_q(), dst.t.ap()[:, c, tt * 512:(tt + 1) * 512], ev[:], [ev], dst)
                else:
                    k.op("act", lambda E, r=r, c=c, tt=tt: E.activation(out=dst[:, c, tt * 512:(tt + 1) * 512], in_=r[:], func=AF.Identity, scale=gain[:, l:l + 1]),
                         [r, gain], [dst])
            return ep
        self.gemmA(w_in, OFF["qa"], 2048, self.hT, ep_norm(self.qaT, self.qn))
        self.gemmA(w_in, OFF["ka"], 512, self.hT, ep_norm(kaT, self.kn))

        def ep_qi(c, tt, ps):
            k.op("act", lambda E, ps=ps, c=c, tt=tt: E.copy(out=self.qiT[:, c, tt * 512:(tt + 1) * 512], in_=ps[:, :]), [ps], [self.qiT])
        self.gemmA(w_in, OFF["qi"], 1024, self.hT, ep_qi)
        wv_ = w_in.t.ap().rearrange("(r p) f -> (r p f)", p=128).rearrange("(kc p n) -> p kc n", p=128, n=D_IN)
        wki = k.sb("wki", [128, KC, 80], BF16)
        k.dma("sp", wki[:], wv_[:, :, OFF["ki"]:OFF["ki"] + 80], [w_in], wki, allow_slow_non_contiguous=True)
        wva = k.sb("wva", [128, KC, 512], BF16)
        k.dma("act", wva[:], wv_[:, :, OFF["va"]:OFF["va"] + 512], [w_in], wva)
        for tt in range(L // 512):
            ps = self.next_ps()
            for kc in range(KC):
                k.op("pe", lambda E, ps=ps, kc=kc, tt=tt: E.matmul(ps[0:64, :], wki[:, kc, 0:64], self.hT[:, kc, tt * 512:(tt + 1) * 512],
                                                                   start=(kc == 0), stop=(kc == KC - 1)), [wki, self.hT], [ps], signal=(kc == KC - 1))
            k.op("act", lambda E, ps=ps, tt=tt: E.copy(out=kiT[:, tt * 512:(tt + 1) * 512], in_=ps[0:64, :]), [ps], [kiT])
        vsb = k.sb("vsb", [128, NB, NKV, 129], BF16)
        k.op("dve", lambda E: E.memset(vsb[:], 1.0), [], [vsb])
        for tb in range(NB):
            ps = self.next_ps()
            for kc in range(KC):
                k.op("pe", lambda E, ps=ps, kc=kc, tb=tb: E.matmul(ps[:, 0:16], self.hT[:, kc, tb * 128:(tb + 1) * 128], wki[:, kc, 64:80],
                                                                   start=(kc == 0), stop=(kc == KC - 1)), [wki, self.hT], [ps], signal=(kc == KC - 1))
            k.op("dve", lambda E, ps=ps, tb=tb: E.tensor_scalar(out=self.wi_sb[:, tb, :], in0=ps[:, 0:16], scalar1=1.0 / 32.0, scalar2=None, op0=ALU.mult), [ps], [self.wi_sb])
            ps = self.next_ps()
            for kc in range(KC):
                k.op("pe", lambda E, ps=ps, kc=kc, tb=tb: E.matmul(ps[:, :], self.hT[:, kc, tb * 128:(tb + 1) * 128], wva[:, kc, :],
                                                                   start=(kc == 0), stop=(kc == KC - 1)), [wva, self.hT], [ps], signal=(kc == KC - 1))
            k.op("act", lambda E, ps=ps, tb=tb: E.copy(out=vsb[:, tb, :, 0:128], in_=ps[:, :].rearrange("p (g e) -> p g e", e=128)), [ps], [vsb])
        ksrc = self.ksrc = k.dram(f"ksrc{l}", [NB * NKV * 128, 128], BF16)
        self.kfull = k.dram(f"kfull{l}", [NCORE * NB * NKV * 128, 128], BF16)
        for g in range(NKV):
            k.dma(self.next_q(), ksrc.t.ap().rearrange("(j g d) s -> d g j s", g=NKV, d=128)[:, g], kaT[:, g, :].rearrange("d (j s) -> d j s", s=128), [kaT], ksrc)
        k.allgather(ksrc, self.kfull)
        vsrc = self.vsrc = k.dram(f"vsrc{l}", [L, NKV * 129], BF16)
        self.vfull = k.dram(f"vfull{l}", [NCORE * L, NKV * 129], BF16)
        k.dma("act", vsrc.t.ap().rearrange("(j s) f -> s j f", s=128), vsb[:].rearrange("s j g e -> s j (g e)"), [vsb], vsrc)
        k.allgather(vsrc, self.vfull)
        kisrc = self.kisrc = k.dram(f"kisrc{l}", [NB * 64, 128], BF16)
        self.kifull = k.dram(f"kifull{l}", [NCORE * NB * 64, 128], BF16)
        k.dma("sp", kisrc.t.ap().rearrange("(j d) s -> d j s", d=64), kiT[:].rearrange("d (j s) -> d j s", s=128), [kiT], kisrc)
        k.allgather(kisrc, self.kifull)
        k.pop()

    def attention(self, l):
        k, L, S = self.k, self.L, self.S
        NB, NCH, NST = L // 128, S // 128, S // 512
        ds = bass.ds
        V = lambda f, r, w: k.op("dve", f, r, w)
        A = lambda f, r, w: k.op("act", f, r, w)
        self.yaT = self.hT
        k.push()
        kiv = self.kifull.t.ap().rearrange("(c d) s -> d c s", d=64)
        sc = k.sb("sc", [128, S], F32)
        junk = k.sb("junkb", [128, S], BF16)
        m01 = junk
        maskT = k.sb("maskT", [128, NCH, 128], BF16)
        diag = k.sb("diag", [128, 16, 128], BF16)
        rl_rot = self.rot("rl", 2, [128, 512], BF16)
        cap = k.sb("cap", [128, 512], F32)
        sm = {n: k.sb("b_" + n, [128, 1], F32) for n in ("lo", "hi", "mid", "cnt", "ge", "d", "qs")}
        kT_rot = self.rot("kTg", 1, [128, S], BF16)
        v_rot = self.rot("vg", 1, [128, NCH, 129], BF16)
        q_rot = self.rot("qg", 2, [128, 4, 128], BF16)
        e_rot = self.rot("E", 2, [128, 512], F32)
        p_rot = self.rot("P", 3, [128, 4, 128], BF16)
        knear = k.sb("kown", [128, NB + 1, NKV, 128], BF16)
        vnear = k.sb("vown", [128, NB + 1, NKV, 129], BF16)
        kiown = k.sb("kiown", [128, NB + 1, 128], BF16)
        mnear = k.sb("mnear", [128, 256], BF16)
        cn = k.sb("cn", [128, 256], F32)
        hasprev = k.sb("hasprev", [128, 1], F32)
        k.dma("sp", cn[:], self.causaln_in.t.ap(), [self.causaln_in], cn)
        k.dma("act", hasprev[:], self.hasprev_in.t.ap(), [self.hasprev_in], hasprev)
        mnT = k.sb("mnT", [128, 2, 128], BF16)
        yatok = k.sb("yatok", [128, D], BF16)
        rden = k.sb("rden", [128, 1], F32)
        kv5 = self.kfull.t.ap().rearrange("(r j g d) s -> d r j g s", r=NCORE, g=NKV, d=128)
        vv5 = self.vfull.t.ap().rearrange("(r j s) (g e) -> s r j g e", r=NCORE, s=128, e=129)
        ki5 = self.kifull.t.ap().rearrange("(r j d) s -> d r j s", r=NCORE, d=64)
        prev = k.prev
        k.dma("pool", knear[:, 0], (lambda E: kv5[:, ds(prev(E), 1), NB - 1].rearrange("d o g s -> d (o g) s")), [self.kfull], knear)
        k.dma("pool", vnear[:, 0], (lambda E: vv5[:, ds(prev(E), 1), NB - 1].rearrange("s o g e -> s (o g) e")), [self.vfull], vnear)
        k.dma("pool", kiown[0:64, 0:1, :], (lambda E: ki5[:, ds(prev(E), 1), NB - 1]), [self.kifull], kiown)
        k.dma("sp", kiown[64:128, 0:1, :], kiown[0:64, 0:1, :], [kiown], kiown)
        for hp in range(2):
            k.dma("sp", kiown[hp * 64:(hp + 1) * 64, 1:, :], self.kisrc.t.ap().rearrange("(j d) s -> d j s", d=64), [self.kisrc], kiown)
        for g in range(NKV):
            k.dma(self.next_q(), knear[:, 1:, g, :], self.ksrc.t.ap().rearrange("(j g d) s -> d g j s", g=NKV, d=128)[:, g], [self.ksrc], knear)
        k.dma("act", vnear[:, 1:].rearrange("s j g e -> s j (g e)"), self.vsrc.t.ap().rearrange("(j s) f -> s j f", s=128), [self.vsrc], vnear)
        kv = self.kfull.t.ap().rearrange("(c g d) s -> g d c s", g=NKV, d=128)
        vv = self.vfull.t.ap().rearrange("(c s) (g e) -> g s c e", s=128, e=129)
        acc = self.ps_aux
        SCALE = float(DH_A) ** -0.5
        for qb in range(NB):
            qs_ = slice(qb * 128, (qb + 1) * 128)
            kiT2 = kT_rot()
            k.dma("sp", kiT2[0:64, :].rearrange("d (c s) -> d c s", s=128), kiv, [self.kifull], kiT2)
            k.dma("act", kiT2[64:128, :].rearrange("d (c s) -> d c s", s=128), kiv, [self.kifull], kiT2)
            for h in range(NH_I):
                V(lambda E, h=h, qb=qb: E.tensor_scalar(out=diag[:, h, :], in0=self.identb[:], scalar1=self.wi_sb[:, qb, h:h + 1], scalar2=None, op0=ALU.mult),
                  [self.identb, self.wi_sb], [diag])
            for st in range(NST):
                pacc = acc[0]
                for h in range(NH_I):
                    hp, pr = h % 2, h // 2
                    ps = self.next_ps()
                    k.op("pe", lambda E, ps=ps, hp=hp, pr=pr, st=st, qs_=qs_: E.matmul(ps[:, :], self.qiT[hp * 64:(hp + 1) * 64, pr, qs_],
                                                                                   kiT2[hp * 64:(hp + 1) * 64, st * 512:(st + 1) * 512], start=True, stop=True),
                         [self.qiT, kiT2], [ps])
                    rl = rl_rot()
                    if h % 2 == 0:
                        A(lambda E, rl=rl, ps=ps: E.activation(out=rl[:], in_=ps[:, :], func=AF.Relu), [ps], [rl])
                    else:
                        V(lambda E, rl=rl, ps=ps: E.tensor_scalar(out=rl[:], in0=ps[:, :], scalar1=0.0, scalar2=None, op0=ALU.max), [ps], [rl])
                    k.op("pe", lambda E, rl=rl, h=h: E.matmul(pacc[:, :], diag[:, h, :], rl[:], start=(h == 0), stop=(h == NH_I - 1)), [diag, rl], [pacc],
                         signal=(h == NH_I - 1))
                V(lambda E, st=st, qb=qb: E.tensor_scalar(out=sm["qs"][:], in0=self.qpos[:, qb:qb + 1], scalar1=float(-st * 512), scalar2=None, op0=ALU.add),
                  [self.qpos], [sm["qs"]])
                V(lambda E: E.tensor_scalar(out=cap[:], in0=self.kpos[:], scalar1=sm["qs"][:, 0:1], scalar2=None, op0=ALU.is_le), [self.kpos, sm["qs"]], [cap])
                V(lambda E: E.tensor_scalar(out=cap[:], in0=cap[:], scalar1=2e9, scalar2=-1e9, op0=ALU.mult, op1=ALU.add), [cap], [cap])
                V(lambda E, st=st: E.tensor_tensor(out=sc[:, st * 512:(st + 1) * 512], in0=pacc[:, :], in1=cap[:], op=ALU.min), [pacc, cap], [sc])
            V(lambda E: E.reduce_max(out=sm["hi"][:], in_=sc[:], axis=AX.X), [sc], [sm["hi"]])
            V(lambda E: E.tensor_scalar(out=junk[:], in0=sc[:], scalar1=-5e8, scalar2=None, op0=ALU.is_le), [sc], [junk])
            V(lambda E: E.scalar_tensor_tensor(out=sc[:], in0=junk[:], scalar=2e9, in1=sc[:], op0=ALU.mult, op1=ALU.add), [junk, sc], [sc])
            V(lambda E: E.tensor_reduce(out=sm["lo"][:], in_=sc[:], axis=AX.X, op=ALU.min), [sc], [sm["lo"]])
            V(lambda E: E.scalar_tensor_tensor(out=sc[:], in0=junk[:], scalar=-2e9, in1=sc[:], op0=ALU.mult, op1=ALU.add), [junk, sc], [sc])
            V(lambda E: E.tensor_scalar(out=sm["hi"][:], in0=sm["hi"][:], scalar1=1e-3, scalar2=None, op0=ALU.add), [sm["hi"]], [sm["hi"]])
            for it in range(22):
                V(lambda E: E.tensor_tensor(out=sm["mid"][:], in0=sm["lo"][:], in1=sm["hi"][:], op=ALU.add), [sm["lo"], sm["hi"]], [sm["mid"]])
                V(lambda E: E.tensor_scalar(out=sm["mid"][:], in0=sm["mid"][:], scalar1=0.5, scalar2=None, op0=ALU.mult), [sm["mid"]], [sm["mid"]])
                V(lambda E: E.tensor_scalar(out=junk[:], in0=sc[:], scalar1=sm["mid"][:, 0:1], scalar2=None, op0=ALU.is_ge), [sc, sm["mid"]], [junk])
                V(lambda E: E.reduce_sum(out=sm["cnt"][:], in_=junk[:], axis=AX.X), [junk], [sm["cnt"]])
                V(lambda E: E.tensor_scalar(out=sm["ge"][:], in0=sm["cnt"][:], scalar1=255.5, scalar2=None, op0=ALU.is_ge), [sm["cnt"]], [sm["ge"]])
                V(lambda E: E.tensor_tensor(out=sm["d"][:], in0=sm["mid"][:], in1=sm["lo"][:], op=ALU.subtract), [sm["mid"], sm["lo"]], [sm["d"]])
                V(lambda E: E.scalar_tensor_tensor(out=sm["lo"][:], in0=sm["d"][:], scalar=sm["ge"][:, 0:1], in1=sm["lo"][:], op0=ALU.mult, op1=ALU.add),
                  [sm["d"], sm["ge"], sm["lo"]], [sm["lo"]])
                V(lambda E: E.tensor_tensor(out=sm["d"][:], in0=sm["mid"][:], in1=sm["hi"][:], op=ALU.subtract), [sm["mid"], sm["hi"]], [sm["d"]])
                V(lambda E: E.tensor_scalar(out=sm["ge"][:], in0=sm["ge"][:], scalar1=-1.0, scalar2=1.0, op0=ALU.mult, op1=ALU.add), [sm["ge"]], [sm["ge"]])
                V(lambda E: E.scalar_tensor_tensor(out=sm["hi"][:], in0=sm["d"][:], scalar=sm["ge"][:, 0:1], in1=sm["hi"][:], op0=ALU.mult, op1=ALU.add),
                  [sm["d"], sm["ge"], sm["hi"]], [sm["hi"]])
            V(lambda E: E.tensor_scalar(out=m01[:], in0=sc[:], scalar1=sm["lo"][:, 0:1], scalar2=None, op0=ALU.is_ge), [sc, sm["lo"]], [m01])
            for c4 in range(NCH // 4):
                for j in range(4):
                    ch = c4 * 4 + j
                    k.op("pe", lambda E, ch=ch, j=j: E.transpose(out=self.psb16[:, j * 128:(j + 1) * 128], in_=m01[:, ch * 128:(ch + 1) * 128], identity=self.identb[:]),
                         [m01, self.identb], [self.psb16], signal=(j == 3))
                A(lambda E, c4=c4: E.copy(out=maskT[:, c4 * 4:(c4 + 1) * 4, :], in_=self.psb16[:, :].rearrange("p (j q) -> p j q", q=128)), [self.psb16], [maskT])
            pnear = acc[1]
            for h in range(NH_I):
                hp, pr = h % 2, h // 2
                ps = self.next_ps()
                k.op("pe", lambda E, ps=ps, hp=hp, pr=pr, qb=qb, qs_=qs_: E.matmul(ps[:, 0:256], self.qiT[hp * 64:(hp + 1) * 64, pr, qs_],
                                                                               kiown[hp * 64:(hp + 1) * 64, qb:qb + 2, :].rearrange("d c s -> d (c s)"), start=True, stop=True),
                     [self.qiT, kiown], [ps])
                rl = rl_rot()
                if h % 2 == 0:
                    A(lambda E, rl=rl, ps=ps: E.activation(out=rl[:, 0:256], in_=ps[:, 0:256], func=AF.Relu), [ps], [rl])
                else:
                    V(lambda E, rl=rl, ps=ps: E.tensor_scalar(out=rl[:, 0:256], in0=ps[:, 0:256], scalar1=0.0, scalar2=None, op0=ALU.max), [ps], [rl])
                k.op("pe", lambda E, rl=rl, h=h: E.matmul(pnear[:, 0:256], diag[:, h, :], rl[:, 0:256], start=(h == 0), stop=(h == NH_I - 1)), [diag, rl], [pnear],
                     signal=(h == NH_I - 1))
            V(lambda E: E.scalar_tensor_tensor(out=mnear[:], in0=pnear[:, 0:256], scalar=sm["lo"][:, 0:1], in1=cn[:], op0=ALU.is_ge, op1=ALU.mult),
              [pnear, sm["lo"], cn], [mnear])
            if qb == 0:
                V(lambda E: E.tensor_scalar(out=mnear[:, 0:128], in0=mnear[:, 0:128], scalar1=hasprev[:, 0:1], scalar2=None, op0=ALU.mult), [mnear, hasprev], [mnear])
            for j in range(2):
                k.op("pe", lambda E, j=j: E.transpose(out=self.psb16[:, j * 128:(j + 1) * 128], in_=mnear[:, j * 128:(j + 1) * 128], identity=self.identb[:]),
                     [mnear, self.identb], [self.psb16], signal=(j == 1))
            A(lambda E: E.copy(out=mnT[:], in_=self.psb16[:, 0:256].rearrange("p (j q) -> p j q", q=128)), [self.psb16], [mnT])
            for g in range(NKV):
                kT, vg = kT_rot(), v_rot()
                k.dma("sp", kT[:].rearrange("d (c s) -> d c s", s=128), kv[g], [self.kfull], kT)
                k.dma("act", vg[:], vv[g], [self.vfull], vg)
                qg = q_rot()
                k.dma("sp", qg[:], self.qaT.t.ap()[:, 4 * g:4 * g + 4, qs_], [self.qaT], qg)
                q_ap = qg[:].rearrange("d h q -> d (h q)")
                for step in range(NCH):
                    ps = self.next_ps()
                    lhs = kT[:, step * 128:(step + 1) * 128]
                    vch = vg[:, step, :]
                    k.op("pe", lambda E, ps=ps, lhs=lhs, q_ap=q_ap: E.matmul(ps[:, :], lhs, q_ap, start=True, stop=True), [kT, qg], [ps])
                    e = e_rot()
                    A(lambda E, e=e, ps=ps: E.activation(out=e[:], in_=ps[:, :], func=AF.Exp, scale=SCALE), [ps], [e])
                    p = p_rot()
                    V(lambda E, e=e, p=p, step=step: E.tensor_tensor(out=p[:], in0=e[:].rearrange("s (h q) -> s h q", q=128),
                                                                     in1=maskT[:, step, :].unsqueeze(1).broadcast_to([128, 4, 128]), op=ALU.mult), [e, maskT], [p])
                    for h in range(4):
                        k.op("pe", lambda E, p=p, h=h, vch=vch, step=step: E.matmul(acc[h][:, 0:129], p[:, h, :], vch, start=(step == 0), stop=False),
                             [p, vg], [acc[h]], signal=(h == 3))
                for jn in range(2):
                    ps = self.next_ps()
                    k.op("pe", lambda E, ps=ps, jn=jn, g=g, q_ap=q_ap, qb=qb: E.matmul(ps[:, :], knear[:, qb + jn, g, :], q_ap, start=True, stop=True), [knear, qg], [ps])
                    e = e_rot()
                    A(lambda E, e=e, ps=ps: E.activation(out=e[:], in_=ps[:, :], func=AF.Exp, scale=SCALE), [ps], [e])
                    V(lambda E, e=e, jn=jn, g=g: E.tensor_tensor(out=e[:].rearrange("s (h q) -> s h q", q=128), in0=e[:].rearrange("s (h q) -> s h q", q=128),
                                                               in1=self.expT[:, jn, 4 * g:4 * g + 4, :], op=ALU.mult), [e, self.expT], [e])
                    p = p_rot()
                    V(lambda E, e=e, p=p, jn=jn: E.tensor_tensor(out=p[:], in0=e[:].rearrange("s (h q) -> s h q", q=128),
                                                                 in1=mnT[:, jn, :].unsqueeze(1).broadcast_to([128, 4, 128]), op=ALU.mult), [e, mnT], [p])
                    for h in range(4):
                        k.op("pe", lambda E, p=p, h=h, jn=jn, g=g, qb=qb: E.matmul(acc[h][:, 0:129], p[:, h, :], vnear[:, qb + jn, g, :], start=False, stop=(jn == 1)),
                             [p, vnear], [acc[h]], signal=(jn == 1 or h == 3))
                for h in range(4):
                    hh = 4 * g + h
                    V(lambda E, h=h: E.reciprocal(out=rden[:], in_=acc[h][:, 128:129]), [acc[h]], [rden])
                    V(lambda E, h=h, hh=hh: E.tensor_scalar(out=yatok[:, hh * 128:(hh + 1) * 128], in0=acc[h][:, 0:128], scalar1=rden[:, 0:1], scalar2=None, op0=ALU.mult),
                      [acc[h], rden], [yatok])
            for f4 in range(KC // 4):
                for j in range(4):
                    fc = f4 * 4 + j
                    k.op("pe", lambda E, fc=fc, j=j: E.transpose(out=self.psb16[:, j * 128:(j + 1) * 128], in_=yatok[:, fc * 128:(fc + 1) * 128], identity=self.identb[:]),
                         [yatok, self.identb], [self.psb16], signal=(j == 3))
                A(lambda E, f4=f4, qs_=qs_: E.copy(out=self.yaT[:, f4 * 4:(f4 + 1) * 4, qs_], in_=self.psb16[:, :].rearrange("p (j q) -> p j q", q=128)), [self.psb16], [self.yaT])
        k.pop()

    def gemmA(self, w, col0, ncols, actT, epilogue):
        k, L = self.k, self.L
        rows, cols = w.shape2
        wv = w.t.ap().rearrange("(r p) f -> (r p f)", p=128).rearrange("(kc p n) -> p kc n", p=128, n=cols)
        for g0 in range(0, ncols, 512):
            gw = min(512, ncols - g0)
            wt = self.wg_rot()
            k.dma(self.next_q(), wt[:, :, 0:gw], wv[:, :, col0 + g0:col0 + g0 + gw], [w], wt)
            for c4 in range(gw // 128):
                for tt in range(L // 512):
                    ps = self.next_ps()
                    for kc in range(KC):
                        k.op("pe", lambda E, ps=ps, wt=wt, kc=kc, c4=c4, tt=tt: E.matmul(
                            ps[:, :], wt[:, kc, c4 * 128:(c4 + 1) * 128], actT[:, kc, tt * 512:(tt + 1) * 512],
                            start=(kc == 0), stop=(kc == KC - 1)), [wt, actT], [ps], signal=(kc == KC - 1))
                    epilogue((g0 // 128) + c4, tt, ps)


def _lay_pk(v):
    v = np.asarray(v)
    lead = v.shape[:-1]
    v2 = v.reshape(-1, v.shape[-1] // 128, 128)
    return np.ascontiguousarray(v2.transpose(2, 0, 1).reshape(128, -1))


def _bucket_onehot():
    out = np.zeros((2, 32, 128, 128), np.float32)
    q = np.arange(128)[:, None]
    s_ = np.arange(128)[None, :]
    for di, delta in enumerate((128, 0)):
        n = np.maximum(delta + q - s_, 0)
        nf = np.maximum(n, 16).astype(np.float32)
        large = 16 + (np.log(nf / 16) / np.float32(np.log(128 / 16)) * 16).astype(np.int32)
        large = np.minimum(large, 31)
        b = np.where(n < 16, n, large)
        for bb in range(32):
            out[di, bb] = (b == bb)
    return out.reshape(2, 32, 128 * 128)


def make_in_maps(inputs, depth, S):
    L = S // NCORE
    x = np.asarray(inputs["x"])[0, :S]
    maps = []
    for i in range(NCORE):
        m = {}
        m["xT"] = np.ascontiguousarray(x[i * L:(i + 1) * L].T)
        m["cvec"] = _lay_pk(inputs["c"][0])
        m["w_ada"] = np.ascontiguousarray(np.concatenate(
            [np.asarray(inputs["w_ada"])[:depth, :, s * D + 0:0] for s in range(0)], axis=-1)) if False else \
            np.ascontiguousarray(np.asarray(inputs["w_ada"])[:depth, :, i * 1536:(i + 1) * 1536])
        ba = np.asarray(inputs["b_ada"])[:depth, i * 1536:(i + 1) * 1536].reshape(depth, 12, 128)
        m["b_ada"] = np.ascontiguousarray(ba.transpose(2, 0, 1).reshape(128, depth * 12))
        m["norm1"] = _lay_pk(np.asarray(inputs["norm1"])[:depth])
        m["norm2"] = _lay_pk(np.asarray(inputs["norm2"])[:depth])
        wr = np.concatenate([np.asarray(inputs["w_grp"])[:depth], np.asarray(inputs["w_exp"])[:depth]], axis=-1)
        m["wr"] = np.ascontiguousarray(wr.reshape(depth, KC, 128, 36).transpose(0, 2, 1, 3).reshape(depth, 128, KC * 36))
        br = np.concatenate([np.asarray(inputs["b_grp"])[:depth], np.asarray(inputs["b_exp"])[:depth]], axis=-1)
        m["br"] = np.ascontiguousarray(np.broadcast_to(br[:, None, :], (depth, 128, 36)))
        m["ident"] = np.eye(128, dtype=np.float32)
        m["causalN"] = np.concatenate([np.ones((128, 128), np.float32), np.tril(np.ones((128, 128), np.float32))], axis=1)
        m["hasprev"] = np.full((128, 1), 0.0 if i == 0 else 1.0, np.float32)
        m["rel_bias"] = np.ascontiguousarray(np.asarray(inputs["rel_bias"], dtype=np.float32))
        m["bkt"] = _bucket_onehot()
        m["qpos"] = (i * L + np.arange(L, dtype=np.float32)).reshape(L // 128, 128).T.copy()
        m["kpos"] = np.ascontiguousarray(np.broadcast_to(np.arange(512, dtype=np.float32)[None], (128, 512)))
        m["qn"] = np.ascontiguousarray(np.asarray(inputs["q_norm"])[:depth].T)
        m["kn"] = np.ascontiguousarray(np.asarray(inputs["k_norm"])[:depth].T)
        m["mn"] = _lay_pk(np.asarray(inputs["mlstm_norm"])[:depth])
        rs_ = D // NCORE
        for nm in ("w_proj_m", "w_proj_a", "w_out"):
            m[nm] = np.ascontiguousarray(np.asarray(inputs[nm])[:depth, i * rs_:(i + 1) * rs_, :]).reshape(depth, -1)
        m["causalT"] = np.triu(np.ones((128, 128), np.float32))
        W = np.asarray(inputs["w_in"])[:depth]
        hs = slice(i * 256, (i + 1) * 256)
        m["whead"] = np.ascontiguousarray(np.concatenate(
            [W[:, :, OFF["qk"]:OFF["qk"] + 2048][:, :, hs], W[:, :, OFF["qk"] + 2048:OFF["qk"] + 4096][:, :, hs],
             W[:, :, OFF["vm"]:OFF["vm"] + 2048][:, :, hs], W[:, :, OFF["om"]:OFF["om"] + 2048][:, :, hs],
             W[:, :, OFF["ip"] + i:OFF["ip"] + i + 1], W[:, :, OFF["fp"] + i:OFF["fp"] + i + 1]], axis=-1)).reshape(depth, -1)
        cwv = np.asarray(inputs["conv_w"])[:depth]
        cq = cwv[:, :, i * 256:(i + 1) * 256].reshape(depth, 4, 2, 128)
        ck = cwv[:, :, 2048 + i * 256:2048 + (i + 1) * 256].reshape(depth, 4, 2, 128)
        m["conv_h"] = np.ascontiguousarray(np.stack([cq, ck], axis=1).transpose(0, 4, 1, 3, 2)).reshape(depth, 128, 16)
        bgv = np.asarray(inputs["b_gate"])[:depth]
        m["bg_h"] = np.ascontiguousarray(np.broadcast_to(np.stack([bgv[:, i], bgv[:, 8 + i]], axis=-1)[:, None, :], (depth, 64, 2)))
        es = np.zeros((32, 32, 128), np.float32)
        es[np.arange(32), np.arange(32), :] = 1.0
        m["esel"] = es.reshape(32, 32 * 128)
        ne = N_EXP // NCORE
        for nm in ("w1", "w3", "w2"):
            m[nm] = np.ascontiguousarray(np.asarray(inputs[nm])[:depth, i * ne:(i + 1) * ne]).reshape(depth, -1)
        rs = D // NCORE
        m["w_in"] = np.ascontiguousarray(np.asarray(inputs["w_in"])[:depth, i * rs:(i + 1) * rs, :]).reshape(depth, -1)
        maps.append(m)
    return maps


def kernel(**inputs):
    depth, S = 4, 8192
    prog = Prog(depth=depth, S=S)
    nc = prog.build()
    maps = make_in_maps(inputs, depth, S)
    res = run_bass_kernel_spmd(nc, maps, core_ids=list(range(NCORE)))
    outs = [res.results[i]["out"].T for i in range(NCORE)]
    return np.concatenate(outs, axis=0)[None].astype(np.float32)
```
